# Optimizing a Trainium2 kernel written in Bass

```python
import jax, jax.numpy as jnp
from jax import lax
import numpy as np

D_MODEL = 1024
BATCH = 16
SEQ = 2048
DEPTH = 2

MLA_HEADS = 6
MLA_NOPE = 64
MLA_ROPE = 32
MLA_V = 64
MLA_Q_RANK = 256
MLA_KV_RANK = 128
ATTN_Q_BLOCK = 128
RET_HEADS = 5
RET_DK = 64
RET_DV = 64
RET_CHUNK = 128
MOBA_HEADS = 5
MOBA_DH = 64
MOBA_BLOCK = 256
MOBA_TOPK = 3
MOBA_Q_CHUNK = 64
ROPE_THETA = 10000.0
LN_EPS = 1e-5
RMS_EPS = 1e-6
D_FF = 2816
N_EXPERTS = 8
TOP_K = 2
D_FF_EXPERT = 3584
DN_ALPHA = (2 * DEPTH) ** 0.25
DN_BETA = (8 * DEPTH) ** -0.25

W_MLA_OUT = MLA_HEADS * MLA_V
W_RET_OUT = RET_HEADS * RET_DV
W_MOBA_OUT = MOBA_HEADS * MOBA_DH
MIX_WIDTH = W_MLA_OUT + W_RET_OUT + W_MOBA_OUT
IN_SIZES = (MLA_Q_RANK, MLA_KV_RANK, MLA_ROPE,
            RET_HEADS * RET_DK, RET_HEADS * RET_DK, RET_HEADS * RET_DV, RET_HEADS * RET_DV,
            MOBA_HEADS * MOBA_DH, MOBA_HEADS * MOBA_DH, MOBA_HEADS * MOBA_DH)
D_IN = sum(IN_SIZES)
N_DENSE = (DEPTH + 1) // 2
N_MOE = DEPTH // 2

kernel_name = "hymba_mla_retnet_moba_deepnorm_moe"


def layer_norm(x, g, b):
    xf = x.astype(jnp.float32)
    mu = jnp.mean(xf, -1, keepdims=True)
    var = jnp.mean(jnp.square(xf - mu), -1, keepdims=True)
    return ((xf - mu) * lax.rsqrt(var + LN_EPS) * g.astype(jnp.float32) + b.astype(jnp.float32)).astype(x.dtype)


def rms_norm(x, g):
    xf = x.astype(jnp.float32)
    y = xf * lax.rsqrt(jnp.mean(jnp.square(xf), -1, keepdims=True) + RMS_EPS)
    return (y * g.astype(jnp.float32)).astype(x.dtype)


def rope_tables(n_pos, dim):
    pos = jnp.arange(n_pos, dtype=jnp.float32)
    inv = ROPE_THETA ** (-jnp.arange(0, dim, 2, dtype=jnp.float32) / dim)
    ang = pos[:, None] * inv[None, :]
    return jnp.cos(ang), jnp.sin(ang)


def apply_rope(x, cos, sin):
    shp = (1, cos.shape[0]) + (1,) * (x.ndim - 3) + (cos.shape[1],)
    c = cos.reshape(shp).astype(x.dtype)
    s = sin.reshape(shp).astype(x.dtype)
    x1, x2 = jnp.split(x, 2, axis=-1)
    return jnp.concatenate([x1 * c - x2 * s, x1 * s + x2 * c], axis=-1)


def mla_attention(c_q, c_kv, k_rope, q_norm_g, kv_norm_g, w_uq, w_ukv, cos, sin):
    B, S, _ = c_q.shape
    H, QB = MLA_HEADS, ATTN_Q_BLOCK
    q = (rms_norm(c_q, q_norm_g) @ w_uq).reshape(B, S, H, MLA_NOPE + MLA_ROPE)
    q_nope = q[..., :MLA_NOPE]
    q_pe = apply_rope(q[..., MLA_NOPE:], cos, sin)
    kv = (rms_norm(c_kv, kv_norm_g) @ w_ukv).reshape(B, S, H, MLA_NOPE + MLA_V)
    k_nope, v = kv[..., :MLA_NOPE], kv[..., MLA_NOPE:]
    k_pe = apply_rope(k_rope, cos, sin)
    scale = (MLA_NOPE + MLA_ROPE) ** -0.5
    nq = S // QB
    qn_blocks = q_nope.reshape(B, nq, QB, H, MLA_NOPE).transpose(1, 0, 2, 3, 4)
    qp_blocks = q_pe.reshape(B, nq, QB, H, MLA_ROPE).transpose(1, 0, 2, 3, 4)
    k_pos = jnp.arange(S)

    def block(args):
        qn, qp, start = args
        s = (jnp.einsum('bqhd,bkhd->bhqk', qn, k_nope)
             + jnp.einsum('bqhd,bkd->bhqk', qp, k_pe)).astype(jnp.float32) * scale
        q_pos = start + jnp.arange(QB)
        s = jnp.where(k_pos[None, :] <= q_pos[:, None], s, -jnp.inf)
        p = jax.nn.softmax(s, axis=-1).astype(v.dtype)
        return jnp.einsum('bhqk,bkhd->bqhd', p, v)

    o = lax.map(block, (qn_blocks, qp_blocks, jnp.arange(nq) * QB))
    return o.transpose(1, 0, 2, 3, 4).reshape(B, S, W_MLA_OUT)


def retention(q, k, v, g, cos, sin):
    B, S, _ = q.shape
    H, C = RET_HEADS, RET_CHUNK
    nc = S // C
    f32 = jnp.float32
    q = apply_rope(q.reshape(B, S, H, RET_DK), cos, sin)
    k = apply_rope(k.reshape(B, S, H, RET_DK), cos, sin) * (RET_DK ** -0.5)
    v = v.reshape(B, S, H, RET_DV)
    log_gamma = jnp.log(1.0 - 2.0 ** (-5.0 - jnp.arange(H, dtype=f32)))
    qc = q.reshape(B, nc, C, H, RET_DK)
    kc = k.reshape(B, nc, C, H, RET_DK)
    vc = v.reshape(B, nc, C, H, RET_DV)
    idx = jnp.arange(C, dtype=f32)
    diff = idx[:, None] - idx[None, :]
    decay_in = jnp.where(diff >= 0, jnp.exp(log_gamma[:, None, None] * jnp.maximum(diff, 0.0)), 0.0)
    intra = jnp.einsum('bnihd,bnjhd->bnhij', qc, kc).astype(f32) * decay_in
    o_intra = jnp.einsum('bnhij,bnjhe->bnihe', intra.astype(v.dtype), vc).astype(f32)
    k_decay = jnp.exp(log_gamma[:, None] * (C - 1.0 - idx))
    kv = jnp.einsum('bnjhd,hj,bnjhe->nbhde', kc.astype(f32), k_decay, vc.astype(f32))
    chunk_decay = jnp.exp(log_gamma * C)[None, :, None, None]

    def step(R, kv_n):
        return R * chunk_decay + kv_n, R

    _, R_prev = lax.scan(step, jnp.zeros((B, H, RET_DK, RET_DV), f32), kv)
    q_decay = jnp.exp(log_gamma[:, None] * (idx + 1.0))
    o_cross = jnp.einsum('bnihd,nbhde,hi->bnihe', qc.astype(f32), R_prev, q_decay)
    o = (o_intra + o_cross).reshape(B, S, H, RET_DV)
    mu = jnp.mean(o, -1, keepdims=True)
    var = jnp.mean(jnp.square(o - mu), -1, keepdims=True)
    o = ((o - mu) * lax.rsqrt(var + RMS_EPS)).reshape(B, S, W_RET_OUT)
    return (jax.nn.silu(g.astype(f32)) * o).astype(q.dtype)


def moba_attention(q, k, v):
    B, S, _ = q.shape
    H, dh, BLK, QC = MOBA_HEADS, MOBA_DH, MOBA_BLOCK, MOBA_Q_CHUNK
    f32 = jnp.float32
    nb = -(-S // BLK)
    Sp = nb * BLK
    pad = Sp - S

    def heads(t):
        t = jnp.pad(t.reshape(B, S, H, dh), ((0, 0), (0, pad), (0, 0), (0, 0)))
        return t.transpose(0, 2, 1, 3)

    q, k, v = heads(q), heads(k), heads(v)
    k_blk = k.reshape(B, H, nb, BLK, dh)
    v_blk = v.reshape(B, H, nb, BLK, dh)
    k_mean = jnp.mean(k_blk.astype(f32), axis=3)
    slopes = 2.0 ** (-8.0 * (jnp.arange(H, dtype=f32) + 1.0) / H)
    n_sel = min(MOBA_TOPK, nb - 1)
    scale = dh ** -0.5
    nqc = Sp // QC
    q_chunks = q.reshape(B, H, nqc, QC, dh).transpose(2, 0, 1, 3, 4)
    bi = jnp.arange(B)[:, None, None, None]
    hi = jnp.arange(H)[None, :, None, None]
    blk_off = jnp.arange(BLK)

    def chunk(args):
        qc, start = args
        blk = start // BLK
        q_pos = start + jnp.arange(QC)
        k_own = lax.dynamic_index_in_dim(k_blk, blk, axis=2, keepdims=False)
        v_own = lax.dynamic_index_in_dim(v_blk, blk, axis=2, keepdims=False)
        own_pos = blk * BLK + blk_off
        s_own = (jnp.einsum('bhqd,bhkd->bhqk', qc, k_own).astype(f32) * scale
                 - slopes[:, None, None] * (q_pos[:, None] - own_pos[None, :]).astype(f32))
        s_own = jnp.where(own_pos[None, :] <= q_pos[:, None], s_own, -jnp.inf)
        if n_sel == 0:
            p = jax.nn.softmax(s_own, axis=-1).astype(v.dtype)
            return jnp.einsum('bhqk,bhkd->bhqd', p, v_own)
        gate = jnp.einsum('bhqd,bhnd->bhqn', qc.astype(f32), k_mean)
        gate = jnp.where(jnp.arange(nb) < blk, gate, -jnp.inf)
        _, sel = lax.top_k(gate, n_sel)
        k_sel = k_blk[bi, hi, sel]
        v_sel = v_blk[bi, hi, sel]
        sel_pos = sel[..., None] * BLK + blk_off
        s_sel = (jnp.einsum('bhqd,bhqnkd->bhqnk', qc, k_sel).astype(f32) * scale
                 - slopes[:, None, None, None] * (q_pos[:, None, None] - sel_pos).astype(f32))
        slot_ok = jnp.arange(n_sel) < blk
        s_sel = jnp.where(slot_ok[:, None], s_sel, -jnp.inf)
        s_all = jnp.concatenate([s_sel.reshape(B, H, QC, n_sel * BLK), s_own], axis=-1)
        p = jax.nn.softmax(s_all, axis=-1).astype(v.dtype)
        p_sel = p[..., :n_sel * BLK].reshape(B, H, QC, n_sel, BLK)
        return (jnp.einsum('bhqnk,bhqnkd->bhqd', p_sel, v_sel)
                + jnp.einsum('bhqk,bhkd->bhqd', p[..., n_sel * BLK:], v_own))

    o = lax.map(chunk, (q_chunks, jnp.arange(nqc) * QC))
    o = o.transpose(1, 0, 3, 2, 4).reshape(B, Sp, W_MOBA_OUT)
    return o[:, :S]


def hybrid_mixer(x, w_in, q_norm_g, kv_norm_g, w_uq, w_ukv, beta_mla, beta_moba, w_o,
                 cos_a, sin_a, cos_b, sin_b):
    h = x @ w_in
    splits = [int(s) for s in np.cumsum(IN_SIZES)[:-1]]
    c_q, c_kv, k_rope, rq, rk, rv, rg, mq, mk, mv = jnp.split(h, splits, axis=-1)
    o_a = rms_norm(mla_attention(c_q, c_kv, k_rope, q_norm_g, kv_norm_g, w_uq, w_ukv, cos_a, sin_a), beta_mla)
    o_b = retention(rq, rk, rv, rg, cos_b, sin_b)
    o_c = rms_norm(moba_attention(mq, mk, mv), beta_moba)
    return jnp.concatenate([o_a, o_b, o_c], axis=-1) @ w_o


def swiglu(x, w_gate, w_up, w_down):
    return (jax.nn.silu(x @ w_gate) * (x @ w_up)) @ w_down


def moe_swiglu(x, w_router, w_gate, w_up, w_down):
    B, S, D = x.shape
    xt = x.reshape(B * S, D)
    logits = (xt @ w_router).astype(jnp.float32)
    top_v, top_i = lax.top_k(logits, TOP_K)
    top_w = jax.nn.softmax(top_v, axis=-1)
    gates = jnp.sum(jax.nn.one_hot(top_i, N_EXPERTS, dtype=jnp.float32) * top_w[..., None], axis=1)
    y = jnp.zeros((B * S, D), jnp.float32)
    for e in range(N_EXPERTS):
        y = y + gates[:, e:e + 1] * swiglu(xt, w_gate[e], w_up[e], w_down[e]).astype(jnp.float32)
    return y.astype(x.dtype).reshape(B, S, D)


def setup_inputs(seed: int = 0) -> dict:
    key = jax.random.key(seed)
    ks = jax.random.split(key, 24)
    f32 = jnp.float32
    nrm = lambda k, shp, s: jax.random.normal(k, shp, f32) * s
    x = jax.random.normal(ks[0], (BATCH, SEQ, D_MODEL), f32)
    col_scale = jnp.concatenate([
        jnp.full((s,), DN_BETA if i in (5, 9) else 1.0, f32) for i, s in enumerate(IN_SIZES)])
    w_in = nrm(ks[1], (DEPTH, D_MODEL, D_IN), D_MODEL ** -0.5) * col_scale
    ukv_scale = jnp.tile(jnp.concatenate([jnp.ones((MLA_NOPE,), f32), jnp.full((MLA_V,), DN_BETA, f32)]), MLA_HEADS)
    return {
        "x": x,
        "ln_emb_g": 1.0 + nrm(ks[2], (D_MODEL,), 0.02),
        "ln_emb_b": nrm(ks[3], (D_MODEL,), 0.02),
        "w_in": w_in,
        "q_norm_g": 1.0 + nrm(ks[4], (DEPTH, MLA_Q_RANK), 0.02),
        "kv_norm_g": 1.0 + nrm(ks[5], (DEPTH, MLA_KV_RANK), 0.02),
        "w_uq": nrm(ks[6], (DEPTH, MLA_Q_RANK, MLA_HEADS * (MLA_NOPE + MLA_ROPE)), MLA_Q_RANK ** -0.5),
        "w_ukv": nrm(ks[7], (DEPTH, MLA_KV_RANK, MLA_HEADS * (MLA_NOPE + MLA_V)), MLA_KV_RANK ** -0.5) * ukv_scale,
        "beta_mla": 1.0 + nrm(ks[8], (DEPTH, W_MLA_OUT), 0.02),
        "beta_moba": 1.0 + nrm(ks[9], (DEPTH, W_MOBA_OUT), 0.02),
        "w_o": nrm(ks[10], (DEPTH, MIX_WIDTH, D_MODEL), MIX_WIDTH ** -0.5 * DN_BETA),
        "ln1_g": 1.0 + nrm(ks[11], (DEPTH, D_MODEL), 0.02),
        "ln1_b": nrm(ks[12], (DEPTH, D_MODEL), 0.02),
        "ffn_w_gate": nrm(ks[13], (N_DENSE, D_MODEL, D_FF), D_MODEL ** -0.5 * DN_BETA),
        "ffn_w_up": nrm(ks[14], (N_DENSE, D_MODEL, D_FF), D_MODEL ** -0.5 * DN_BETA),
        "ffn_w_down": nrm(ks[15], (N_DENSE, D_FF, D_MODEL), D_FF ** -0.5 * DN_BETA),
        "router": nrm(ks[16], (N_MOE, D_MODEL, N_EXPERTS), D_MODEL ** -0.5),
        "exp_w_gate": nrm(ks[17], (N_MOE, N_EXPERTS, D_MODEL, D_FF_EXPERT), D_MODEL ** -0.5 * DN_BETA),
        "exp_w_up": nrm(ks[18], (N_MOE, N_EXPERTS, D_MODEL, D_FF_EXPERT), D_MODEL ** -0.5 * DN_BETA),
        "exp_w_down": nrm(ks[19], (N_MOE, N_EXPERTS, D_FF_EXPERT, D_MODEL), D_FF_EXPERT ** -0.5 * DN_BETA),
        "ln2_g": 1.0 + nrm(ks[20], (DEPTH, D_MODEL), 0.02),
        "ln2_b": nrm(ks[21], (DEPTH, D_MODEL), 0.02),
    }


def reference(x, ln_emb_g, ln_emb_b, w_in, q_norm_g, kv_norm_g, w_uq, w_ukv, beta_mla, beta_moba,
              w_o, ln1_g, ln1_b, ffn_w_gate, ffn_w_up, ffn_w_down, router, exp_w_gate, exp_w_up,
              exp_w_down, ln2_g, ln2_b):
    S = x.shape[1]
    cos_a, sin_a = rope_tables(S, MLA_ROPE)
    cos_b, sin_b = rope_tables(S, RET_DK)
    x = layer_norm(x, ln_emb_g, ln_emb_b)
    for l in range(DEPTH):
        mix = hybrid_mixer(x, w_in[l], q_norm_g[l], kv_norm_g[l], w_uq[l], w_ukv[l],
                           beta_mla[l], beta_moba[l], w_o[l], cos_a, sin_a, cos_b, sin_b)
        x = layer_norm(DN_ALPHA * x + mix, ln1_g[l], ln1_b[l])
        if l % 2 == 0:
            j = l // 2
            f = swiglu(x, ffn_w_gate[j], ffn_w_up[j], ffn_w_down[j])
        else:
            j = l // 2
            f = moe_swiglu(x, router[j], exp_w_gate[j], exp_w_up[j], exp_w_down[j])
        x = layer_norm(DN_ALPHA * x + f, ln2_g[l], ln2_b[l])
    return x
```

```python
import os
import numpy as np
from contextlib import ExitStack
import concourse.bass as bass
import concourse.mybir as mybir
from concourse.bass_utils import run_bass_kernel_spmd

F32 = mybir.dt.float32
BF16 = mybir.dt.bfloat16
AF = mybir.ActivationFunctionType
ALU = mybir.AluOpType
AX = mybir.AxisListType

D = 1024
S = 2048
DEPTH = 2
NCORES = 8
D_IN = 2656
D_FF = 2816
NEXP = 8
D_FFE = 3584
ALPHA = float((2 * DEPTH) ** 0.25)
LN_EPS = 1e-5
RMS_EPS = 1e-6
MASKVAL = -1.0e6
ATT_LAG = 3
NT = 4
NPL = 48
PA_COLS, PR_COLS, PC_COLS = 448, 384, 192
OFF_A = 0
OFF_R = 8 * PA_COLS
OFF_C = OFF_R + 5 * 8 * PR_COLS
WIN_TOT = OFF_C + 5 * 8 * PC_COLS
GAMMA = [1.0 - 2.0 ** (-5.0 - h) for h in range(5)]
SLOPES = [2.0 ** (-8.0 * (h + 1.0) / 5.0) for h in range(5)]


def _kcp(w):
    n = w.shape[1]
    return np.ascontiguousarray(w.reshape(8, 128, n).transpose(1, 0, 2))


def _swap(w):
    h = w.shape[1] // 2
    return np.concatenate([w[:, h:], w[:, :h]], axis=1)


def ffn_groups():
    gl = []
    f = 0
    while f < D_FF // 128:
        g = min(4, D_FF // 128 - f)
        gl.append((0, None, f, g))
        f += g
    for e in range(NEXP):
        for f in range(0, D_FFE // 128, 4):
            gl.append((1, e, f, 4))
    return gl


def prep_shared(inp):
    f32 = np.float32
    out = {}
    P = np.zeros((128, 16 + DEPTH * NPL), f32)

    def pc(v):
        return v.reshape(-1, 128).T

    P[:, 0:8] = pc(inp["ln_emb_g"])
    P[:, 8:16] = pc(inp["ln_emb_b"])
    for l in range(DEPTH):
        b = 16 + l * NPL
        P[:, b:b + 8] = pc(inp["ln1_g"][l])
        P[:, b + 8:b + 16] = pc(inp["ln1_b"][l])
        P[:, b + 16:b + 24] = pc(inp["ln2_g"][l])
        P[:, b + 24:b + 32] = pc(inp["ln2_b"][l])
        P[:, b + 32:b + 34] = pc(inp["q_norm_g"][l])
        P[:, b + 34:b + 35] = pc(inp["kv_norm_g"][l])
        beta = np.ones((1024,), f32)
        beta[0:384] = inp["beta_mla"][l]
        beta[704:1024] = inp["beta_moba"][l]
        P[:, b + 35:b + 43] = pc(beta)
    out["params"] = P
    winp = np.zeros((DEPTH, 128, WIN_TOT), f32)
    wuq = np.zeros((DEPTH, 128, 2 * 768), f32)
    wukv = np.zeros((DEPTH, 128, 1152), f32)
    wo = np.zeros((DEPTH, 128, 8 * 1024), f32)
    for l in range(DEPTH):
        w = inp["w_in"][l]
        pa = np.concatenate([w[:, 0:256], w[:, 256:384], w[:, 384:416], _swap(w[:, 384:416])], axis=1)
        winp[l, :, OFF_A:OFF_A + 8 * PA_COLS] = _kcp(pa).reshape(128, -1)
        for h in range(5):
            q = w[:, 416 + 64 * h:416 + 64 * h + 64]
            k = w[:, 736 + 64 * h:736 + 64 * h + 64]
            v = w[:, 1056 + 64 * h:1056 + 64 * h + 64]
            g = w[:, 1376 + 64 * h:1376 + 64 * h + 64]
            pr = np.concatenate([q, _swap(q), k, _swap(k), v, g], axis=1)
            o = OFF_R + h * 8 * PR_COLS
            winp[l, :, o:o + 8 * PR_COLS] = _kcp(pr).reshape(128, -1)
            mq = w[:, 1696 + 64 * h:1696 + 64 * h + 64]
            mk = w[:, 2016 + 64 * h:2016 + 64 * h + 64]
            mv = w[:, 2336 + 64 * h:2336 + 64 * h + 64]
            pcc = np.concatenate([mq, mk, mv], axis=1)
            o = OFF_C + h * 8 * PC_COLS
            winp[l, :, o:o + 8 * PC_COLS] = _kcp(pcc).reshape(128, -1)
        uq = inp["w_uq"][l]
        cols = []
        for h in range(6):
            nope = uq[:, h * 96:h * 96 + 64]
            rope = uq[:, h * 96 + 64:h * 96 + 96]
            cols += [rope, _swap(rope), nope]
        uqp = np.concatenate(cols, axis=1)
        wuq[l] = uqp.reshape(2, 128, 768).transpose(1, 0, 2).reshape(128, -1)
        ukv = inp["w_ukv"][l]
        kc = []
        vc = []
        for h in range(6):
            kc += [np.zeros((128, 64), f32), ukv[:, h * 128:h * 128 + 64]]
            vc += [ukv[:, h * 128 + 64:h * 128 + 128]]
        wukv[l] = np.concatenate(kc + vc, axis=1)
        wo[l] = _kcp(inp["w_o"][l]).reshape(128, -1)
    out["winp"] = winp
    out["wuq"] = wuq
    out["wukv"] = wukv
    out["wo"] = wo
    gl = ffn_groups()
    ffw = np.zeros((len(gl), 128, 12288), f32)
    for gi, (lay, e, f0, G) in enumerate(gl):
        if e is None:
            wg, wu, wd = inp["ffn_w_gate"][0], inp["ffn_w_up"][0], inp["ffn_w_down"][0]
        else:
            wg, wu, wd = inp["exp_w_gate"][0, e], inp["exp_w_up"][0, e], inp["exp_w_down"][0, e]
        c0, c1 = f0 * 128, (f0 + G) * 128
        ffw[gi, :, 0:8 * G * 128] = _kcp(wg[:, c0:c1]).reshape(128, -1)
        ffw[gi, :, 4096:4096 + 8 * G * 128] = _kcp(wu[:, c0:c1]).reshape(128, -1)
        ffw[gi, :, 8192:8192 + G * 1024] = wd[c0:c1, :].reshape(G, 128, 1024).transpose(1, 0, 2).reshape(128, -1)
    out["ffw"] = ffw
    out["router"] = _kcp(inp["router"][0]).reshape(128, 64)
    pos = np.arange(S, dtype=np.float64)
    inv_m = 10000.0 ** (-np.arange(0, 32, 2, dtype=np.float64) / 32)
    ang = inv_m[:, None] * pos[None, :]
    out["ropem"] = np.concatenate([np.cos(ang), np.cos(ang), -np.sin(ang), np.sin(ang)], axis=0).astype(f32)
    inv_r = 10000.0 ** (-np.arange(0, 64, 2, dtype=np.float64) / 64)
    ang = inv_r[:, None] * pos[None, :]
    out["roper"] = np.concatenate([np.cos(ang), np.cos(ang), -np.sin(ang), np.sin(ang)], axis=0).astype(f32)
    i = np.arange(128, dtype=np.float64)
    dec = np.zeros((5, 128, 512), f32)
    qdec = np.zeros((5, 64, 512), f32)
    kdec = np.zeros((128, 5), f32)
    for h in range(5):
        lg = np.log(GAMMA[h])
        dm = np.where(i[None, :] >= i[:, None], np.exp(-lg * (i[:, None] + 1.0)), 0.0) * 0.125
        dec[h] = np.tile(dm, (1, 4))
        qd = np.exp(lg * (i + 1.0))
        qdec[h] = np.tile(qd[None, :], (64, 4))
        kdec[:, h] = np.exp(lg * (127.0 - i)) * 0.125
    out["dec"] = dec
    out["qdec"] = qdec
    out["kdec"] = kdec
    al = np.zeros((128, 5 * 32), f32)
    for h in range(5):
        for c in range(32):
            al[:, h * 32 + c] = SLOPES[h] * (i + (c - 16) * 128.0)
    out["alibi"] = al
    b1 = np.zeros((8, S), f32)
    for n in range(8):
        b1[n, n * 256:(n + 1) * 256] = 1.0
    out["blk1h"] = b1
    cm = np.zeros((128, 256), f32)
    cm[:, 0:128] = np.eye(128)
    cm[:, 128:256] = (i[None, :] >= i[:, None]).astype(f32)
    out["cmisc"] = cm
    mc = np.zeros((128, 128), f32)
    for t in range(8):
        bq = (8 + t) // 2
        for n in range(8):
            mc[:, t * 8 + n] = 1.0 if n < bq else 0.0
            mc[:, 64 + t * 8 + n] = 0.0 if n < bq else -1.0e30
    out["mobc"] = mc
    return out


class TK:
    def __init__(self, nc, es):
        self.nc = nc
        self.es = es
        self.eng = {"pe": nc.tensor, "act": nc.scalar, "dve": nc.vector, "pool": nc.gpsimd, "sp": nc.sync}
        self.sem = {k: es.enter_context(nc.semaphore("s_" + k)) for k in self.eng}
        self.cnt = {k: 0 for k in self.eng}
        self.seen = {k: {} for k in self.eng}
        self.lastw = {}
        self.readers = {}
        self.dsem = {}

    def _wait(self, e, ev):
        sem, val, src = ev
        if src == "pe" and e == "pe":
            return
        if self.seen[e].get(src, 0) >= val:
            return
        self.eng[e].wait_ge(sem, val)
        self.seen[e][src] = val

    def _deps(self, e, reads, writes):
        for k in reads:
            ev = self.lastw.get(k)
            if ev:
                self._wait(e, ev)
            if isinstance(k, tuple) and k[0] == "ps":
                for ev in self.readers.get(k, {}).values():
                    if ev[2] != e:
                        self._wait(e, ev)
        for k in writes:
            ev = self.lastw.get(k)
            if ev:
                self._wait(e, ev)
            for ev in self.readers.get(k, {}).values():
                self._wait(e, ev)

    def _commit(self, ev, reads, writes):
        for k in reads:
            self.readers.setdefault(k, {})[ev[2]] = ev
        for k in writes:
            self.lastw[k] = ev
            self.readers[k] = {}

    def op(self, e, fn, reads=(), writes=()):
        self._deps(e, reads, writes)
        inst = fn()
        self.cnt[e] += 1
        inst.then_inc(self.sem[e], 1)
        self._commit((self.sem[e], self.cnt[e], e), reads, writes)

    def dma(self, q, out, in_, reads, writes, skey, **kw):
        self._deps(q, reads, writes)
        if skey not in self.dsem:
            self.dsem[skey] = [self.es.enter_context(self.nc.semaphore("d%d" % len(self.dsem))), 0]
        d = self.dsem[skey]
        src = "d:" + str(skey)
        if d[1] > 0:
            self._wait(q, (d[0], d[1], src))
        d[1] += 16
        self.eng[q].dma_start(out=out, in_=in_, **kw).then_inc(d[0], 16)
        self._commit((d[0], d[1], src), reads, writes)

    def finish(self):
        for skey, d in self.dsem.items():
            self._wait("sp", (d[0], d[1], "d:" + str(skey)))
        for e in ("pe", "act", "dve", "pool"):
            if self.cnt[e]:
                self._wait("sp", (self.sem[e], self.cnt[e], e))


MARKS = []


def build_program(nseq=2, nlayers=DEPTH, stop=None, taps=()):
    nc = bass.Bass("TRN2", target_bir_lowering=False)
    gl = ffn_groups()
    dr = {}

    def din(name, shape):
        dr[name] = nc.dram_tensor(name, list(shape), F32, kind="ExternalInput").ap()
        return dr[name]

    xin = din("xin", (nseq, S, D))
    params_d = din("params", (128, 16 + DEPTH * NPL))
    winp_d = din("winp", (DEPTH, 128, WIN_TOT))
    wuq_d = din("wuq", (DEPTH, 128, 1536))
    wukv_d = din("wukv", (DEPTH, 128, 1152))
    wo_d = din("wo", (DEPTH, 128, 8192))
    ffw_d = din("ffw", (len(gl), 128, 12288))
    router_d = din("router", (128, 64))
    ropem_d = din("ropem", (64, S))
    roper_d = din("roper", (128, S))
    dec_d = din("dec", (5, 128, 512))
    qdec_d = din("qdec", (5, 64, 512))
    kdec_d = din("kdec", (128, 5))
    alibi_d = din("alibi", (128, 160))
    blk1h_d = din("blk1h", (8, S))
    cmisc_d = din("cmisc", (128, 256))
    mobc_d = din("mobc", (128, 128))
    yout = nc.dram_tensor("yout", [nseq, S, D], F32, kind="ExternalOutput").ap()
    tap_d = {}
    for tname, tshape in taps:
        tap_d[tname] = nc.dram_tensor(tname, list(tshape), F32, kind="ExternalOutput").ap()

    es = ExitStack()
    with es:
        def sb(name, shape, dt):
            return es.enter_context(nc.sbuf_tensor(name, list(shape), dt))

        NSLAB = 27
        X32 = sb("X32", (128, 8, S), F32)
        SL = sb("SL", (128, NSLAB, S), BF16)
        ROPE = sb("ROPE", (128, S), F32)
        NSC = 7
        SC = sb("SC", (128, NSC, 512), F32)
        PAR = sb("PAR", (128, 16 + DEPTH * NPL), F32)
        PARA = sb("PARA", (128, 16 + DEPTH * NPL), F32)
        IDB = sb("IDB", (128, 128), BF16)
        TRI = sb("TRI", (128, 128), BF16)
        IDF = sb("IDF", (128, 128), F32)
        ONF = sb("ONF", (128, 128), F32)
        ONB = sb("ONB", (128, 128), BF16)
        ALI = sb("ALI", (128, 160), F32)
        KDEC = sb("KDEC", (128, 5), F32)
        DECB = sb("DECB", (128, 512), F32)
        QDECB = sb("QDECB", (64, 512), F32)
        ROUT = sb("ROUT", (128, 64), F32)
        GATE = sb("GATE", (128, 16, 8), F32)
        SM = sb("SM", (128, 128), F32)
        MOBC = sb("MOBC", (128, 128), F32)
        RALL = sb("RALL", (64, 17, 64), F32)
        KS = sb("KS", (64, 8), F32)
        PS = [es.enter_context(nc.psum_tensor("PS%d" % i, [128, 512], F32)) for i in range(8)]
        tk = TK(nc, es)
        V, A, G, T = nc.vector, nc.scalar, nc.gpsimd, nc.tensor

        rot = {}

        def nxt(name, lst):
            i = rot.get(name, 0)
            rot[name] = i + 1
            return lst[i % len(lst)]

        def psb(grp):
            banks = {"st": [0, 1, 2, 3], "ot": [4, 5], "pj": [6, 7], "aux": [7, 6], "gu": [0, 1, 2, 3], "y": [4, 5, 6]}[grp]
            b = nxt("ps_" + grp, banks)
            return PS[b], ("ps", b)

        def psf(b):
            return PS[b], ("ps", b)

        def scb(grp="r", lst=(4, 5, 6)):
            i = nxt("sc_" + grp, list(lst))
            return SC[:, i, :], ("sc", i)

        def scf(i):
            return SC[:, i, :], ("sc", i)

        def slab(i):
            return SL[:, i, :]

        def slabs(i, n):
            return SL[:, i:i + n, :].rearrange("p a b -> p (a b)")

        XBF0, MIX0 = 0, 8
        PT_SLAB = 16
        XBF = SL[:, 0:8, :]
        MIXT = SL[:, 8:16, :]

        def kx32(c, tt):
            return ("x32", c, tt)

        def kxbf(c, tt):
            return ("xbf", c, tt)

        tk.dma("sp", PAR[:], params_d[:, :], [], ["par"], "c")
        tk.dma("sp", IDF[:], cmisc_d[:, 0:128], [], ["idf"], "c")
        tk.dma("pool", IDB[:], cmisc_d[:, 0:128], [], ["idb"], "c")
        tk.dma("pool", TRI[:], cmisc_d[:, 128:256], [], ["tri"], "c")
        tk.dma("sp", ALI[:], alibi_d[:, :], [], ["ali"], "c")
        tk.dma("sp", KDEC[:], kdec_d[:, :], [], ["kdec"], "c")
        tk.dma("sp", ROUT[:], router_d[:, :], [], ["rout"], "c")
        tk.dma("sp", MOBC[:], mobc_d[:, :], [], ["mobc"], "c")
        tk.op("dve", lambda: V.memset(ONF[:], 1.0), [], ["onf"])
        tk.op("dve", lambda: V.tensor_scalar(out=PARA[:], in0=PAR[:], scalar1=ALPHA, scalar2=None, op0=ALU.mult), ["par"], ["par"])
        tk.op("dve", lambda: V.memset(ONB[:], 1.0), [], ["onb"])

        def tap(name, src_ap, keys, dst=None):
            if name in tap_d:
                tk.dma("sp", tap_d[name] if dst is None else dst, src_ap, keys, [("tap", name, str(dst is None))], ("tap", name))

        def layer_norm(gcol, bcol, out_scale):
            for tt in range(NT):
                ln_tile(tt, gcol, bcol, out_scale)

        def ln_tile(tt, gcol, bcol, out_scale):
            if True:
                ts = slice(tt * 512, (tt + 1) * 512)
                pm, kpm = psf(5)
                pq, kpq = psf(7)
                for c in range(8):
                    tk.op("pe", lambda c=c: T.matmul(pm[:], ONF[:], X32[:, c, ts], start=(c == 0), stop=(c == 7)),
                          [kx32(c, tt), "onf"], [kpm])
                for c in range(8):
                    pi = nxt("pt", [0, 1, 2, 3])
                    sq = SL[:, PT_SLAB, pi * 512:(pi + 1) * 512]
                    ksq = ("pt", pi)
                    tk.op("act", lambda c=c, sq=sq: A.activation(out=sq, in_=X32[:, c, ts], func=AF.Square),
                          [kx32(c, tt)], [ksq])
                    tk.op("pe", lambda c=c, sq=sq: T.matmul(pq[:], ONB[:], sq, start=(c == 0), stop=(c == 7)),
                          [ksq, "onb"], [kpq])
                mean, kmean = scf(0)
                var, kvar = scf(1)
                rs, krs = scf(2)
                nb, knb = scf(3)
                tk.op("act", lambda: A.activation(out=mean, in_=pm[:], func=AF.Copy, scale=1.0 / D), [kpm], [kmean])
                tk.op("dve", lambda: V.tensor_tensor(out=var, in0=mean, in1=mean, op=ALU.mult), [kmean], [kvar])
                tk.op("dve", lambda: V.scalar_tensor_tensor(out=var, in0=pq[:], scalar=1.0 / D, in1=var,
                                                            op0=ALU.mult, op1=ALU.subtract), [kpq, kvar], [kvar])
                tk.op("act", lambda: A.activation(out=var, in_=var, func=AF.Ln, bias=LN_EPS, scale=1.0), [kvar], [kvar])
                tk.op("act", lambda: A.activation(out=rs, in_=var, func=AF.Exp, scale=-0.5), [kvar], [krs])
                tk.op("dve", lambda: V.scalar_tensor_tensor(out=nb, in0=mean, scalar=-1.0, in1=rs,
                                                            op0=ALU.mult, op1=ALU.mult), [kmean, krs], [knb])
                PS_ = PAR if out_scale == 1.0 else PARA
                for c in range(8):
                    t1, kt1 = scb()
                    gc = PAR[:, gcol + c:gcol + c + 1]
                    bc = PAR[:, bcol + c:bcol + c + 1]
                    gs = PS_[:, gcol + c:gcol + c + 1]
                    bs_ = PS_[:, bcol + c:bcol + c + 1]
                    e1, e2 = ("dve", "dve") if c % 4 != 3 else ("pool", "pool")
                    E1 = V if e1 == "dve" else G
                    tk.op(e1, lambda: E1.tensor_tensor(out=t1, in0=X32[:, c, ts], in1=rs, op=ALU.mult),
                          [kx32(c, tt), krs], [kt1])
                    tk.op(e2, lambda: E1.tensor_tensor(out=t1, in0=t1, in1=nb, op=ALU.add), [knb, kt1], [kt1])
                    tk.op("act", lambda: A.activation(out=XBF[:, c, ts], in_=t1, func=AF.Identity, bias=bc, scale=gc),
                          [kt1, "par"], [kxbf(c, tt)])
                    tk.op("act", lambda: A.activation(out=X32[:, c, ts], in_=t1, func=AF.Identity, bias=bs_, scale=gs),
                          [kt1, "par"], [kx32(c, tt)])

        def attention(QT, kq, KT, kk, VA, kva, scale, bias_h, nsub, mix_chunk, mix_p0, bcol_unused=None):
            units = [(qt, j) for qt in range(NT) for j in range(4 * qt + 4)]
            state = {}

            def emit_st(qt, j):
                m = max(0, j - 4 * qt)
                c0 = m * 128
                st, kst = psb("st")
                tk.op("pe", lambda: T.matmul(st[:, c0:512], KT[:, j * 128:(j + 1) * 128],
                                             QT[:, qt * 512 + c0:(qt + 1) * 512], start=True, stop=True),
                      [kq, kk], [kst])
                pi = nxt("pt", [0, 1, 2, 3])
                pt = SL[:, PT_SLAB, pi * 512:(pi + 1) * 512]
                kpt = ("pt", pi)
                if bias_h is None:
                    tk.op("act", lambda: A.activation(out=pt[:, c0:512], in_=st[:, c0:512], func=AF.Exp, scale=scale),
                          [kst], [kpt])
                elif nsub == 1:
                    col = bias_h * 32 + 16 + (j - 4 * qt)
                    tk.op("act", lambda: A.activation(out=pt[:, c0:512], in_=st[:, c0:512], func=AF.Exp, scale=scale,
                                                      bias=ALI[:, col:col + 1]), [kst, "ali"], [kpt])
                else:
                    for mm in range(m, 4):
                        col = bias_h * 32 + 16 + (j - 4 * qt - mm)
                        tk.op("act", lambda mm=mm, col=col: A.activation(
                            out=pt[:, mm * 128:(mm + 1) * 128], in_=st[:, mm * 128:(mm + 1) * 128], func=AF.Exp,
                            scale=scale, bias=ALI[:, col:col + 1]), [kst, "ali"], [kpt])
                if j >= 4 * qt:
                    tk.op("pool", lambda: G.tensor_tensor(out=pt[:, c0:c0 + 128], in0=pt[:, c0:c0 + 128], in1=TRI[:],
                                                          op=ALU.mult), [kpt, "tri"], [kpt])
                return (qt, j, c0, pt, kpt)

            def emit_pv(u):
                qt, j, c0, pt, kpt = u
                if j == 0:
                    state["ot"] = psb("ot")
                ot, kot = state["ot"]
                nj = 4 * qt + 4
                tk.op("pe", lambda: T.matmul(ot[:, c0:512], VA[:, j, :], pt[:, c0:512], start=(j == 0), stop=(j == nj - 1),
                                             skip_group_check=True),
                      [kva, kpt], [kot])
                if j == nj - 1:
                    rc, krc = scb()
                    tk.op("dve", lambda: V.reciprocal(out=rc[0:64, :], in_=ot[64:128, :]), [kot], [krc])
                    ts = slice(qt * 512, (qt + 1) * 512)
                    tk.op("dve", lambda: V.tensor_tensor(out=MIXT[mix_p0:mix_p0 + 64, mix_chunk, ts], in0=ot[0:64, :],
                                                         in1=rc[0:64, :], op=ALU.mult),
                          [kot, krc], [("mix", mix_chunk, qt, mix_p0), ("sl", 8 + mix_chunk)])

            pend = []
            for (qt, j) in units:
                pend.append(emit_st(qt, j))
                if len(pend) > ATT_LAG:
                    emit_pv(pend.pop(0))
                yield
            while pend:
                emit_pv(pend.pop(0))

        def run_chain(items):
            p0, m0, h0 = items[0]
            for _ in p0(h0, h0 % 2):
                pass
            for i, (p, m, h) in enumerate(items):
                nx = None
                if i + 1 < len(items):
                    pn, mn, hn = items[i + 1]
                    nx = pn(hn, hn % 2)
                for _ in m(h, h % 2):
                    if nx is not None:
                        next(nx, None)
                if nx is not None:
                    for _ in nx:
                        pass

        def run_heads(nh, prep, attn):
            run_chain([(prep, attn, h) for h in range(nh)])

        PT_SLAB = 16

        def mix_loc(ch):
            return ch // 128, ch % 128

        def rms_bc(ps_list_keys, nparts, inv_n, out_rs, krs_out):
            pass

        def mm(out, lhsT, rhs, start, stop, reads, writes, **kw):
            tk.op("pe", lambda: T.matmul(out, lhsT, rhs, start=start, stop=stop, **kw), reads, writes)

        def mark(name):
            MARKS.append((name, tk.cnt["pe"]))

        def mixer(s, l, pb):
            fuse_ln = not (stop in ("mla1", "mla", "ret", "moba"))
            mark("mixer_start s%d l%d" % (s, l))
            KSL = lambda i: ("sl", i)
            XK = lambda tt: [kxbf(c, tt) for c in range(8)]
            CQN = SL[:, 17:19, :]
            CKVN = SL[:, 19, :]
            QT = SL[:, 20, :]
            KT = SL[:, 21, :]
            VA = SL[:, 22, :].rearrange("p (a b) -> p a b", a=16)
            PACKA = slabs(23, 2)[:, 0:3584].rearrange("p (k c) -> p k c", c=PA_COLS)
            WUQ = SL[:, 25, 0:1536].rearrange("p (k h c) -> p k h c", k=2, h=6)
            WUKV = SL[:, 26, 0:1152]
            tk.dma("pool", slabs(23, 2)[:, 0:3584], winp_d[l, :, OFF_A:OFF_A + 3584], [], [KSL(23), KSL(24)],
                   "wA", max_dma_last_dim=4096)
            stq = SC[:, 0:3, :].rearrange("p a b -> p (a b)")
            kstq = [("sc", 0), ("sc", 1), ("sc", 2)]
            tk.dma("sp", stq, wuq_d[l, :, :], [], kstq, "wB")
            for kc in range(2):
                tk.op("dve", lambda: V.tensor_scalar(out=SL[:, 25, kc * 768:(kc + 1) * 768], in0=stq[:, kc * 768:(kc + 1) * 768],
                                                     scalar1=PAR[:, pb + 32 + kc:pb + 33 + kc], scalar2=None, op0=ALU.mult),
                      kstq + ["par"], [KSL(25)])
            stk = SC[:, 3:6, :].rearrange("p a b -> p (a b)")
            kstk = [("sc", 3), ("sc", 4), ("sc", 5)]
            tk.dma("sp", stk[:, 0:1152], wukv_d[l, :, :], [], kstk, "wC")
            tk.op("dve", lambda: V.tensor_scalar(out=WUKV, in0=stk[:, 0:1152], scalar1=PAR[:, pb + 34:pb + 35], scalar2=None,
                                                 op0=ALU.mult), kstk + ["par"], [KSL(26)])
            tk.dma("sp", ROPE[0:64, :], ropem_d[:, :], [], ["rope"], "wD")
            tk.op("pool", lambda: G.memset(SL[32:64, 21, :], 0.0), [], [KSL(21)])
            tk.op("pool", lambda: G.memset(SL[32:64, 20, :], 0.0), [], [KSL(20)])
            tk.op("pool", lambda: G.memset(VA[:, :, 64:128], 1.0), [], [KSL(22)])

            def rope_to(ps_ap, kps, nrow, ts, out_ap, out_keys, also=None):
                a_, ka = scf(0)
                b_, kb = scf(1)
                tk.op("dve", lambda: V.tensor_tensor(out=a_[0:nrow, :], in0=ps_ap[0:nrow, :], in1=ROPE[0:nrow, ts], op=ALU.mult),
                      [kps, "rope"], [ka])
                tk.op("dve", lambda: V.tensor_tensor(out=b_[0:nrow, :], in0=ps_ap[nrow:2 * nrow, :], in1=ROPE[nrow:2 * nrow, ts],
                                                     op=ALU.mult), [kps, "rope"], [kb])
                if also is None:
                    tk.op("pool", lambda: G.tensor_tensor(out=out_ap, in0=a_[0:nrow, :], in1=b_[0:nrow, :], op=ALU.add),
                          [ka, kb], out_keys)
                else:
                    c_, kc_ = scf(2)
                    tk.op("pool", lambda: G.tensor_tensor(out=c_[0:nrow, :], in0=a_[0:nrow, :], in1=b_[0:nrow, :], op=ALU.add),
                          [ka, kb], [kc_])
                    tk.op("act", lambda: A.activation(out=out_ap, in_=c_[0:nrow, :], func=AF.Copy), [kc_], out_keys)
                    o2, k2, tab, ktab = also
                    tk.op("dve", lambda: V.tensor_tensor(out=o2, in0=c_[0:nrow, :], in1=tab, op=ALU.mult), [kc_, ktab], k2)

            for tt in range(NT):
                ts = slice(tt * 512, (tt + 1) * 512)
                pcs = []
                ssq, kssq = psf(6)
                for cc in range(2):
                    pc_, kpc = psf(4 + cc)
                    for kc in range(8):
                        mm(pc_[:], PACKA[:, kc, cc * 128:(cc + 1) * 128], XBF[:, kc, ts], kc == 0, kc == 7,
                           [KSL(23), KSL(24)] + XK(tt), [kpc])
                    sq, ksq = scb()
                    tk.op("act", lambda: A.activation(out=sq, in_=pc_[:], func=AF.Square), [kpc], [ksq])
                    mm(ssq[:], ONF[:], sq, cc == 0, cc == 1, [ksq, "onf"], [kssq])
                    pcs.append((pc_, kpc))
                sd, ksd = scf(0)
                tk.op("act", lambda: A.activation(out=sd, in_=ssq[:], func=AF.Ln, bias=RMS_EPS, scale=1.0 / 256), [kssq], [ksd])
                tk.op("act", lambda: A.activation(out=sd, in_=sd, func=AF.Exp, scale=-0.5), [ksd], [ksd])
                for cc in range(2):
                    pc_, kpc = pcs[cc]
                    tk.op("dve", lambda: V.tensor_tensor(out=CQN[:, cc, ts], in0=pc_[:], in1=sd, op=ALU.mult), [kpc, ksd],
                          [("cqn", cc, tt)])
                pk_, kpk = psf(0)
                for kc in range(8):
                    mm(pk_[:], PACKA[:, kc, 256:384], XBF[:, kc, ts], kc == 0, kc == 7, [KSL(23), KSL(24)] + XK(tt), [kpk])
                sq, ksq = scb()
                tk.op("act", lambda: A.activation(out=sq, in_=pk_[:], func=AF.Square), [kpk], [ksq])
                ss2, kss2 = psf(7)
                mm(ss2[:], ONF[:], sq, True, True, [ksq, "onf"], [kss2])
                sd2, ksd2 = scf(1)
                tk.op("act", lambda: A.activation(out=sd2, in_=ss2[:], func=AF.Ln, bias=RMS_EPS, scale=1.0 / 128), [kss2], [ksd2])
                tk.op("act", lambda: A.activation(out=sd2, in_=sd2, func=AF.Exp, scale=-0.5), [ksd2], [ksd2])
                tk.op("dve", lambda: V.tensor_tensor(out=CKVN[:, ts], in0=pk_[:], in1=sd2, op=ALU.mult), [kpk, ksd2],
                      [("ckvn", tt)])
                pr_, kpr = psf(1)
                for kc in range(8):
                    mm(pr_[0:64, :], PACKA[:, kc, 384:448], XBF[:, kc, ts], kc == 0, kc == 7, [KSL(23), KSL(24)] + XK(tt), [kpr])
                rope_to(pr_, kpr, 32, ts, KT[0:32, ts], [KSL(21)])

            def dump():
                for c in range(8):
                    for tt in range(NT):
                        ts = slice(tt * 512, (tt + 1) * 512)
                        tk.op("act", lambda: A.activation(out=X32[:, c, ts], in_=MIXT[:, c, ts], func=AF.Copy),
                              [("mix", c, tt, 0), ("mix", c, tt, 64)], [kx32(c, tt)])
                return True
            mark("mla_heads")
            if stop == "mla1":
                return dump()
            SETS = [(20, 21, 22), (13, 14, 15)]
            tk.op("act", lambda: A.activation(out=SL[0:32, 14, :], in_=SL[0:32, 21, :], func=AF.Copy), [KSL(21)], [KSL(14)])
            tk.op("pool", lambda: G.memset(SL[32:64, 14, :], 0.0), [], [KSL(14)])
            tk.op("pool", lambda: G.memset(SL[32:64, 13, :], 0.0), [], [KSL(13)])
            tk.op("pool", lambda: G.memset(SL[:, 15, :].rearrange("p (a b) -> p a b", a=16)[:, :, 64:128], 1.0), [], [KSL(15)])

            def mla_prep(h, bs):
                qs, ks_, vs = SETS[bs]
                QTb, KTb = SL[:, qs, :], SL[:, ks_, :]
                VAb = SL[:, vs, :].rearrange("p (a b) -> p a b", a=16)
                for tt in range(NT):
                    ts = slice(tt * 512, (tt + 1) * 512)
                    pk_, kpk = psb("pj")
                    mm(pk_[:], WUKV[:, h * 128:(h + 1) * 128], CKVN[:, ts], True, True, [KSL(26), ("ckvn", tt)], [kpk])
                    tk.op("act", lambda: A.activation(out=KTb[64:128, ts], in_=pk_[64:128, :], func=AF.Copy), [kpk], [KSL(ks_)])
                    pq_, kpq = psb("pj")
                    for kc in range(2):
                        mm(pq_[:], WUQ[:, kc, h, :], CQN[:, kc, ts], kc == 0, kc == 1, [KSL(25), ("cqn", kc, tt)], [kpq])
                    tk.op("act", lambda: A.activation(out=QTb[64:128, ts], in_=pq_[64:128, :], func=AF.Copy), [kpq], [KSL(qs)])
                    rope_to(pq_, kpq, 32, ts, QTb[0:32, ts], [KSL(qs)])
                    yield
                for half in range(2):
                    pv_, kpv = psb("aux")
                    for jj in range(8):
                        j = half * 8 + jj
                        mm(pv_[:, jj * 64:(jj + 1) * 64], CKVN[:, j * 128:(j + 1) * 128], WUKV[:, 768 + h * 64:768 + (h + 1) * 64],
                           True, True, [KSL(26), ("ckvn", j // 4)], [kpv], skip_group_check=True)
                    tk.op("dve", lambda: V.tensor_copy(out=VAb[:, half * 8:(half + 1) * 8, 0:64],
                                                       in_=pv_[:].rearrange("p (a b) -> p a b", a=8)), [kpv], [KSL(vs)])
                    yield

            def mla_attn(h, bs):
                qs, ks_, vs = SETS[bs]
                return attention(SL[:, qs, :], KSL(qs), SL[:, ks_, :], KSL(ks_),
                                 SL[:, vs, :].rearrange("p (a b) -> p a b", a=16), KSL(vs),
                                 float(96 ** -0.5), None, 1, h // 2, (h % 2) * 64)

            mark("ret")
            RSETS = [((17, 18), 19, 20, 22, 21), ((24, 25), 26, 14, 15, 23)]
            def ret_prep(h, bs):
                if h == 0:
                    tk.dma("sp", ROPE[:], roper_d[:, :], [], ["rope"], "wD")
                (p0_, p1_), qs, ks_, vs, rs_ = RSETS[bs]
                PACKR = slabs(p0_, 2)[:, 0:3072].rearrange("p (k c) -> p k c", c=PR_COLS)
                QTr, KTr = SL[:, qs, :], SL[:, ks_, :]
                VK = SL[:, vs, :].rearrange("p (a b) -> p a b", a=16)
                RB = SL[0:64, rs_, 0:1024].rearrange("p (a b) -> p a b", a=16)
                cd = float(GAMMA[h] ** 128)
                o = OFF_R + h * 3072
                kw_ = [KSL(p0_), KSL(p1_)]
                tk.dma("pool", slabs(p0_, 2)[:, 0:3072], winp_d[l, :, o:o + 3072], [], kw_, "wA", max_dma_last_dim=4096)
                for tt in range(NT):
                    ts = slice(tt * 512, (tt + 1) * 512)
                    pq_, kpq = psb("pj")
                    for kc in range(8):
                        mm(pq_[:], PACKR[:, kc, 0:128], XBF[:, kc, ts], kc == 0, kc == 7, kw_ + XK(tt), [kpq])
                    rope_to(pq_, kpq, 64, ts, QTr[0:64, ts], [KSL(qs)])
                    yield
                    pk_, kpk = psb("pj")
                    for kc in range(8):
                        mm(pk_[:], PACKR[:, kc, 128:256], XBF[:, kc, ts], kc == 0, kc == 7, kw_ + XK(tt), [kpk])
                    rope_to(pk_, kpk, 64, ts, KTr[0:64, ts], [KSL(ks_)])
                    yield
                for half in range(2):
                    pv_, kpv = psb("aux")
                    for jj in range(8):
                        j = half * 8 + jj
                        for kc in range(8):
                            mm(pv_[:, jj * 64:(jj + 1) * 64], XBF[:, kc, j * 128:(j + 1) * 128], PACKR[:, kc, 256:320],
                               kc == 0, kc == 7, kw_ + [kxbf(kc, j // 4)], [kpv], skip_group_check=True)
                    tk.op("act", lambda: A.activation(out=VK[:, half * 8:(half + 1) * 8, 0:64],
                                                      in_=pv_[:].rearrange("p (a b) -> p a b", a=8), func=AF.Copy),
                          [kpv], [KSL(vs)])
                    yield
                    pd_, kpd = psb("aux")
                    for jj in range(8):
                        j = half * 8 + jj
                        mm(pd_[:, jj * 64:(jj + 1) * 64], KTr[0:64, j * 128:(j + 1) * 128], IDB[0:64, 0:64], True, True,
                           [KSL(ks_), "idb"], [kpd], skip_group_check=True)
                    tk.op("dve", lambda: V.tensor_scalar(out=VK[:, half * 8:(half + 1) * 8, 64:128],
                                                         in0=pd_[:].rearrange("p (a b) -> p a b", a=8),
                                                         scalar1=KDEC[:, h:h + 1], scalar2=None, op0=ALU.mult),
                          [kpd, "kdec"], [KSL(vs)])
                    yield
                pkv = [psb("aux"), psb("aux")]
                for n in range(16):
                    pb_, kpb = pkv[n // 8]
                    mm(pb_[0:64, (n % 8) * 64:(n % 8 + 1) * 64], VK[:, n, 64:128], VK[:, n, 0:64], True, True, [KSL(vs)], [kpb],
                       skip_group_check=True)
                tk.op("dve", lambda: V.memset(RALL[:, 0, :], 0.0), [], ["rall"])
                for n in range(15):
                    pb_, kpb = pkv[n // 8]
                    tk.op("dve", lambda: V.scalar_tensor_tensor(out=RALL[:, n + 1, :], in0=RALL[:, n, :], scalar=cd,
                                                                in1=pb_[0:64, (n % 8) * 64:(n % 8 + 1) * 64],
                                                                op0=ALU.mult, op1=ALU.add), [kpb, "rall"], ["rall"])
                tk.op("act", lambda: A.activation(out=RB, in_=RALL[:, 0:16, :], func=AF.Copy), ["rall"], [KSL(rs_)])
                yield

            def ret_main(h, bs):
                (p0_, p1_), qs, ks_, vs, rs_ = RSETS[bs]
                PACKR = slabs(p0_, 2)[:, 0:3072].rearrange("p (k c) -> p k c", c=PR_COLS)
                QTr, KTr = SL[:, qs, :], SL[:, ks_, :]
                VK = SL[:, vs, :].rearrange("p (a b) -> p a b", a=16)
                RB = SL[0:64, rs_, 0:1024].rearrange("p (a b) -> p a b", a=16)
                kw_ = [KSL(p0_), KSL(p1_)]
                ch = 384 + 64 * h
                mchunk, mp0 = ch // 128, ch % 128
                tk.dma("sp", DECB[:], dec_d[h, :, :], [], ["decb"], "wD")
                tk.dma("sp", QDECB[:], qdec_d[h, :, :], [], ["qdecb"], "wD")
                for tt in range(NT):
                    ts = slice(tt * 512, (tt + 1) * 512)
                    pa_, kpa = psb("st")
                    for nn in range(4):
                        n = tt * 4 + nn
                        mm(pa_[:, nn * 128:(nn + 1) * 128], KTr[0:64, n * 128:(n + 1) * 128], QTr[0:64, n * 128:(n + 1) * 128],
                           True, True, [KSL(qs), KSL(ks_)], [kpa], skip_group_check=True)
                    pi = nxt("pt", [0, 1, 2, 3])
                    at = SL[:, PT_SLAB, pi * 512:(pi + 1) * 512]
                    kat = ("pt", pi)
                    tk.op("dve", lambda: V.tensor_tensor(out=at, in0=pa_[:], in1=DECB[:], op=ALU.mult), [kpa, "decb"], [kat])
                    pg_, kpg = psb("pj")
                    for kc in range(8):
                        mm(pg_[0:64, :], PACKR[:, kc, 320:384], XBF[:, kc, ts], kc == 0, kc == 7, kw_ + XK(tt), [kpg])
                    sg, ksg = scf(6)
                    tk.op("act", lambda: A.activation(out=sg[0:64, :], in_=pg_[0:64, :], func=AF.Silu), [kpg], [ksg])
                    yield
                    po_, kpo = psb("ot")
                    for nn in range(4):
                        n = tt * 4 + nn
                        cs = slice(nn * 128, (nn + 1) * 128)
                        mm(po_[0:64, cs], VK[:, n, 0:64], at[:, cs], True, False, [KSL(vs), kat], [kpo], skip_group_check=True)
                        mm(po_[0:64, cs], RB[:, n, :], QTr[0:64, n * 128:(n + 1) * 128], False, True, [KSL(rs_), KSL(qs)], [kpo],
                           skip_group_check=True)
                    o32, ko32 = scf(3)
                    cen, kcen = scf(4)
                    zz, kzz = scf(5)
                    tk.op("dve", lambda: V.tensor_tensor(out=o32[0:64, :], in0=po_[0:64, :], in1=QDECB[:], op=ALU.mult),
                          [kpo, "qdecb"], [ko32])
                    yield
                    pm_, kpm = psb("aux")
                    mm(pm_[0:64, :], ONF[0:64, 0:64], o32[0:64, :], True, True, [ko32, "onf"], [kpm])
                    tk.op("dve", lambda: V.scalar_tensor_tensor(out=cen[0:64, :], in0=pm_[0:64, :], scalar=-1.0 / 64,
                                                                in1=o32[0:64, :], op0=ALU.mult, op1=ALU.add),
                          [kpm, ko32], [kcen])
                    pi2 = nxt("pt", [0, 1, 2, 3])
                    zb = SL[:, PT_SLAB, pi2 * 512:(pi2 + 1) * 512]
                    kzb = ("pt", pi2)
                    tk.op("act", lambda: A.activation(out=zb[0:64, :], in_=cen[0:64, :], func=AF.Square), [kcen], [kzb])
                    yield
                    pv2, kpv2 = psb("aux")
                    mm(pv2[0:64, :], ONB[0:64, 0:64], zb[0:64, :], True, True, [kzb, "onb"], [kpv2])
                    tk.op("act", lambda: A.activation(out=zz[0:64, :], in_=pv2[0:64, :], func=AF.Ln, bias=RMS_EPS,
                                                      scale=1.0 / 64), [kpv2], [kzz])
                    tk.op("act", lambda: A.activation(out=zz[0:64, :], in_=zz[0:64, :], func=AF.Exp, scale=-0.5), [kzz], [kzz])
                    tk.op("dve", lambda: V.tensor_tensor(out=cen[0:64, :], in0=cen[0:64, :], in1=zz[0:64, :], op=ALU.mult),
                          [kcen, kzz], [kcen])
                    tk.op("pool", lambda: G.tensor_tensor(out=MIXT[mp0:mp0 + 64, mchunk, ts], in0=cen[0:64, :], in1=sg[0:64, :],
                                                          op=ALU.mult), [kcen, ksg], [("mix", mchunk, tt, mp0), ("sl", 8 + mchunk)])
                    yield

            run_chain([(mla_prep, mla_attn, h) for h in range(6)] + [(ret_prep, ret_main, h) for h in range(5)])

            mark("moba")
            if stop == "ret":
                return dump()
            MSETS = [(20, 21, 22, 17, 0), (23, 24, 25, 18, 2)]
            for (qs, ks_, vs, ps_, sc0) in MSETS:
                tk.op("pool", lambda: G.memset(SL[64:96, ks_, :], 0.0), [], [KSL(ks_)])
                tk.dma("pool", SL[64:72, ks_, :], blk1h_d[:, :], [], [KSL(ks_)], "wA", max_dma_last_dim=4096)
                tk.op("pool", lambda: G.memset(SL[64:96, qs, :], 0.0), [], [KSL(qs)])
                tk.op("pool", lambda: G.memset(SL[:, vs, :].rearrange("p (a b) -> p a b", a=16)[:, :, 64:128], 1.0), [], [KSL(vs)])
            MB3 = SL[:, 26, 0:1024].rearrange("p (t c) -> p t c", t=8)
            tk.op("pool", lambda: G.memset(MB3, 0.0), [], ["mb", KSL(26)])

            def moba_prep(h, bs):
                qs, ks_, vs, ps_, sc0 = MSETS[bs]
                QTb, KTb = SL[:, qs, :], SL[:, ks_, :]
                VAb = SL[:, vs, :].rearrange("p (a b) -> p a b", a=16)
                PACKC = SL[:, ps_, 0:1536].rearrange("p (k c) -> p k c", c=PC_COLS)
                o = OFF_C + h * 1536
                tk.dma("pool", SL[:, ps_, 0:1536], winp_d[l, :, o:o + 1536], [], [KSL(ps_)], "wA")
                kw_ = [KSL(ps_)]
                q32 = {}
                for tt in range(NT):
                    ts = slice(tt * 512, (tt + 1) * 512)
                    pq_, kpq = psb("pj")
                    for kc in range(8):
                        mm(pq_[:, :], PACKC[:, kc, 0:128], XBF[:, kc, ts], kc == 0, kc == 7, kw_ + XK(tt), [kpq])
                    tk.op("dve", lambda: V.tensor_copy(out=QTb[0:64, ts], in_=pq_[0:64, :]), [kpq], [KSL(qs)])
                    tk.op("dve", lambda: V.tensor_copy(out=KTb[0:64, ts], in_=pq_[64:128, :]), [kpq], [KSL(ks_)])
                    if tt >= 2:
                        q32[tt] = scf(sc0 + tt - 2)
                        tk.op("dve", lambda: V.tensor_copy(out=q32[tt][0][0:64, :], in_=pq_[0:64, :]), [kpq], [q32[tt][1]])
                    tk.op("dve", lambda: V.tensor_reduce(out=KS[:, 2 * tt:2 * tt + 2],
                                                         in_=pq_[64:128, :].rearrange("p (a b) -> p a b", a=2),
                                                         axis=AX.X, op=ALU.add), [kpq], ["ks"])
                    yield
                for half in range(2):
                    pv_, kpv = psb("aux")
                    for jj in range(8):
                        j = half * 8 + jj
                        for kc in range(8):
                            mm(pv_[:, jj * 64:(jj + 1) * 64], XBF[:, kc, j * 128:(j + 1) * 128], PACKC[:, kc, 128:192],
                               kc == 0, kc == 7, kw_ + [kxbf(kc, j // 4)], [kpv], skip_group_check=True)
                    tk.op("dve", lambda: V.tensor_copy(out=VAb[:, half * 8:(half + 1) * 8, 0:64],
                                                       in_=pv_[:].rearrange("p (a b) -> p a b", a=8)), [kpv], [KSL(vs)])
                    yield
                pg_, kpg = psb("aux")
                for t in range(8):
                    i = 8 + t
                    tt = i // 4
                    mm(pg_[:, t * 8:(t + 1) * 8], q32[tt][0][0:64, (i % 4) * 128:(i % 4 + 1) * 128], KS[:, 0:8], True, True,
                       [q32[tt][1], "ks"], [kpg], skip_group_check=True)
                gp = SM[:, 0:64]
                gp3 = gp.rearrange("p (t n) -> p t n", t=8)
                cnt3 = SM[:, 64:128].rearrange("p (t n) -> p t n", t=8)
                tk.op("dve", lambda: V.tensor_tensor(out=gp, in0=pg_[:, 0:64], in1=MOBC[:, 0:64], op=ALU.mult), [kpg, "mobc"], ["sm0"])
                tk.op("dve", lambda: V.tensor_tensor(out=gp, in0=gp, in1=MOBC[:, 64:128], op=ALU.add), ["sm0", "mobc"], ["sm0"])
                cmp_, kcmp = scf(6)
                cmp4 = cmp_.rearrange("p (t n m) -> p t n m", t=8, n=8)
                tk.op("dve", lambda: V.tensor_tensor(out=cmp4, in0=gp3.unsqueeze(2).broadcast_to([128, 8, 8, 8]),
                                                     in1=gp3.unsqueeze(3).broadcast_to([128, 8, 8, 8]), op=ALU.is_gt),
                      ["sm0"], [kcmp])
                tk.op("dve", lambda: V.tensor_reduce(out=cnt3, in_=cmp4, axis=AX.X, op=ALU.add), [kcmp], ["sm1"])
                tk.op("dve", lambda: V.tensor_scalar(out=SM[:, 64:128], in0=SM[:, 64:128], scalar1=3.0, scalar2=MASKVAL,
                                                     op0=ALU.is_ge, op1=ALU.mult), ["sm1"], ["sm1"])
                tk.op("dve", lambda: V.tensor_tensor(out=MB3[:, :, 64:72], in0=cnt3,
                                                     in1=MOBC[:, 0:64].rearrange("p (t n) -> p t n", t=8), op=ALU.mult),
                      ["sm1", "mobc"], ["mb"])
                for k2 in range(2):
                    pm_, kpm = psb("aux")
                    for t4 in range(4):
                        t = k2 * 4 + t4
                        mm(pm_[:, t4 * 128:(t4 + 1) * 128], MB3[:, t, :], IDB[:], True, True, ["mb", "idb"], [kpm],
                           skip_group_check=True)
                    tk.op("dve", lambda: V.tensor_copy(out=QTb[64:72, (8 + 4 * k2) * 128:(12 + 4 * k2) * 128], in_=pm_[64:72, :]),
                          [kpm], [KSL(qs)])
                yield

            def moba_attn(h, bs):
                qs, ks_, vs, ps_, sc0 = MSETS[bs]
                ch = 704 + 64 * h
                return attention(SL[0:96, qs, :], KSL(qs), SL[0:96, ks_, :], KSL(ks_),
                                 SL[:, vs, :].rearrange("p (a b) -> p a b", a=16), KSL(vs),
                                 0.125, h, 4 if h == 0 else 1, ch // 128, ch % 128)

            run_heads(5, moba_prep, moba_attn)

            mark("wo")
            if stop == "moba":
                return dump()
            WO = SL[:, 17:21, :].rearrange("p a b -> p (a b)").rearrange("p (k c) -> p k c", k=8)
            for kc in range(8):
                pair = nxt("wostg", [0, 2])
                stg = SC[:, pair:pair + 2, :].rearrange("p a b -> p (a b)")
                kst = [("sc", pair), ("sc", pair + 1)]
                tk.dma("sp", stg, wo_d[l, :, kc * 1024:(kc + 1) * 1024], [], kst, ("wB" if pair == 0 else "wC"))
                tk.op("dve", lambda: V.tensor_scalar(out=WO[:, kc, :], in0=stg, scalar1=PAR[:, pb + 35 + kc:pb + 36 + kc],
                                                     scalar2=None, op0=ALU.mult), kst + ["par"], [KSL(17 + kc // 2)])
            kwo = [KSL(17), KSL(18), KSL(19), KSL(20)]
            for tt in range(NT + 1):
                if tt >= 1 and fuse_ln:
                    ln_tile(tt - 1, pb + 0, pb + 8, ALPHA)
                    if l % 2 == 1:
                        router_tile(tt - 1)
                if tt == NT:
                    break
                ts = slice(tt * 512, (tt + 1) * 512)
                pa_, kpa = psf(6)
                pc_, kpc = psf(7)

                def sqmm(ps_, kps, c, p0, p1, first, last):
                    pi = nxt("sq21", [0, 1, 2, 3])
                    sq = SL[:, 21, pi * 512:(pi + 1) * 512]
                    ksq = ("sq21", pi)
                    tk.op("act", lambda: A.activation(out=sq[p0:p1, :], in_=MIXT[p0:p1, c, ts], func=AF.Square),
                          [("mix", c, tt, 0), ("mix", c, tt, 64), ("sl", 8 + c)], [ksq, ("sl", 21)])
                    mm(ps_[:], ONB[p0:p1, :], sq[p0:p1, :], first, last, [ksq, "onb"], [kps])
                for c in range(3):
                    sqmm(pa_, kpa, c, 0, 128, c == 0, c == 2)
                sqmm(pc_, kpc, 5, 64, 128, True, False)
                sqmm(pc_, kpc, 6, 0, 128, False, False)
                sqmm(pc_, kpc, 7, 0, 128, False, True)
                ra, kra = scf(4)
                rc, krc = scf(5)
                tk.op("act", lambda: A.activation(out=ra, in_=pa_[:], func=AF.Ln, bias=RMS_EPS, scale=1.0 / 384), [kpa], [kra])
                tk.op("act", lambda: A.activation(out=rc, in_=pc_[:], func=AF.Ln, bias=RMS_EPS, scale=1.0 / 320), [kpc], [krc])
                tk.op("act", lambda: A.activation(out=ra, in_=ra, func=AF.Exp, scale=-0.5), [kra], [kra])
                tk.op("act", lambda: A.activation(out=rc, in_=rc, func=AF.Exp, scale=-0.5), [krc], [krc])
                for d in range(8):
                    ds_ = slice(d * 128, (d + 1) * 128)
                    b3 = [nxt("w3", [0, 1, 2, 3, 4, 5]) for _ in range(3)]
                    (pA, kA), (pB, kB), (pC, kC) = [psf(b) for b in b3]
                    mk = lambda c: [("mix", c, tt, 0), ("mix", c, tt, 64), ("sl", 8 + c)]
                    for c in range(3):
                        mm(pA[:], WO[:, c, ds_], MIXT[:, c, ts], c == 0, c == 2, kwo + mk(c), [kA])
                    mm(pB[:], WO[:, 3, ds_], MIXT[:, 3, ts], True, False, kwo + mk(3), [kB])
                    mm(pB[:], WO[:, 4, ds_], MIXT[:, 4, ts], False, False, kwo + mk(4), [kB])
                    mm(pB[:], WO[0:64, 5, ds_], MIXT[0:64, 5, ts], False, True, kwo + mk(5), [kB])
                    mm(pC[:], WO[64:128, 5, ds_], MIXT[64:128, 5, ts], True, False, kwo + mk(5), [kC])
                    mm(pC[:], WO[:, 6, ds_], MIXT[:, 6, ts], False, False, kwo + mk(6), [kC])
                    mm(pC[:], WO[:, 7, ds_], MIXT[:, 7, ts], False, True, kwo + mk(7), [kC])
                    tk.op("dve", lambda: V.tensor_tensor(out=X32[:, d, ts], in0=pB[:], in1=X32[:, d, ts], op=ALU.add),
                          [kB, kx32(d, tt)], [kx32(d, tt)])
                    t1, kt1 = scb("wo", (0, 1, 2, 3))
                    tk.op("dve", lambda: V.tensor_tensor(out=t1, in0=pA[:], in1=ra, op=ALU.mult), [kA, kra], [kt1])
                    tk.op("pool", lambda: G.tensor_tensor(out=X32[:, d, ts], in0=X32[:, d, ts], in1=t1, op=ALU.add),
                          [kt1, kx32(d, tt)], [kx32(d, tt)])
                    t2, kt2 = scb("wo", (0, 1, 2, 3))
                    tk.op("dve", lambda: V.tensor_tensor(out=t2, in0=pC[:], in1=rc, op=ALU.mult), [kC, krc], [kt2])
                    tk.op("pool", lambda: G.tensor_tensor(out=X32[:, d, ts], in0=X32[:, d, ts], in1=t2, op=ALU.add),
                          [kt2, kx32(d, tt)], [kx32(d, tt)])

        def router_tile(tt):
            for j in range(4 * tt, 4 * tt + 4):
                pl, kpl = psb("aux")
                for kc in range(8):
                    mm(pl[:, 0:8], X32[:, kc, j * 128:(j + 1) * 128], ROUT[:, kc * 8:(kc + 1) * 8], kc == 0, kc == 7,
                       [kx32(kc, j // 4), "rout"], [kpl])
                lg = SM[:, 16:24]
                mx = SM[:, 24:32]
                tk.op("dve", lambda: V.tensor_scalar(out=lg, in0=pl[:, 0:8], scalar1=1.0 / ALPHA, scalar2=None, op0=ALU.mult),
                      [kpl], ["smr"])
                tk.op("dve", lambda: V.max(out=mx, in_=lg), ["smr"], ["smr"])
                tk.op("dve", lambda: V.tensor_tensor(out=SM[:, 32:33], in0=mx[:, 1:2], in1=mx[:, 0:1], op=ALU.subtract),
                      ["smr"], ["smr"])
                tk.op("act", lambda: A.activation(out=SM[:, 33:34], in_=SM[:, 32:33], func=AF.Exp), ["smr"], ["smr"])
                tk.op("dve", lambda: V.tensor_scalar(out=SM[:, 34:35], in0=SM[:, 33:34], scalar1=1.0, scalar2=None, op0=ALU.add),
                      ["smr"], ["smr"])
                tk.op("dve", lambda: V.reciprocal(out=SM[:, 35:36], in_=SM[:, 34:35]), ["smr"], ["smr"])
                tk.op("dve", lambda: V.tensor_tensor(out=SM[:, 36:37], in0=SM[:, 33:34], in1=SM[:, 35:36], op=ALU.mult),
                      ["smr"], ["smr"])
                tk.op("dve", lambda: V.tensor_scalar(out=SM[:, 40:48], in0=lg, scalar1=mx[:, 0:1], scalar2=SM[:, 35:36],
                                                     op0=ALU.is_equal, op1=ALU.mult), ["smr"], ["smr"])
                tk.op("dve", lambda: V.tensor_scalar(out=SM[:, 48:56], in0=lg, scalar1=mx[:, 1:2], scalar2=SM[:, 36:37],
                                                     op0=ALU.is_equal, op1=ALU.mult), ["smr"], ["smr"])
                tk.op("dve", lambda: V.tensor_tensor(out=GATE[:, j, :], in0=SM[:, 40:48], in1=SM[:, 48:56], op=ALU.add),
                      ["smr"], ["gate"])


        def ffn(s, l, pb):
            ln2_scale = 1.0 if l == DEPTH - 1 else ALPHA
            mark("ffn s%d l%d" % (s, l))
            KSL = lambda i: ("sl", i)
            groups = [(gi, e, G) for gi, (lay, e, f0, G) in enumerate(gl) if lay == l]
            moe = (l == 1)
            WGb = [slabs(8, 2), slabs(10, 2)]
            WUb = [slabs(12, 2), slabs(14, 2)]
            WDb = [slabs(17, 2), slabs(19, 2)]
            kWG = [[KSL(8), KSL(9)], [KSL(10), KSL(11)]]
            kWU = [[KSL(12), KSL(13)], [KSL(14), KSL(15)]]
            kWD = [[KSL(17), KSL(18)], [KSL(19), KSL(20)]]
            GBC = ROPE

            def HT(set_, g):
                return SL[:, 21 + set_, g * 512:(g + 1) * 512]

            def build_gbc(e):
                for q4 in range(4):
                    pgb, kpgb = psb("aux")
                    dg, kdg = scf(6)
                    for jj in range(4):
                        j = q4 * 4 + jj
                        cs = slice(jj * 128, (jj + 1) * 128)
                        tk.op("dve", lambda: V.tensor_scalar(out=dg[:, cs], in0=IDF[:], scalar1=GATE[:, j, e:e + 1], scalar2=None,
                                                             op0=ALU.mult), ["gate", "idf"], [kdg])
                        mm(pgb[:, cs], ONF[:], dg[:, cs], True, True, [kdg, "onf"], [kpgb], skip_group_check=True)
                    tk.op("act", lambda: A.activation(out=GBC[:, q4 * 512:(q4 + 1) * 512], in_=pgb[:], func=AF.Copy),
                          [kpgb], ["rope"])

            def load(gidx):
                gi, e, G = groups[gidx]
                b = gidx % 2
                n = 8 * G * 128
                tk.dma("pool", WGb[b][:, 0:n], ffw_d[gi, :, 0:n], [], kWG[b], ("fg", b), max_dma_last_dim=4096)
                tk.dma("pool", WUb[b][:, 0:n], ffw_d[gi, :, 4096:4096 + n], [], kWU[b], ("fu", b), max_dma_last_dim=4096)
                tk.dma("pool", WDb[b][:, 0:G * 1024], ffw_d[gi, :, 8192:8192 + G * 1024], [], kWD[b], ("fd", b),
                       max_dma_last_dim=4096)

            units = [(gidx, tt) for gidx in range(len(groups)) for tt in range(NT)]

            def GU(ui):
                gidx, tt = units[ui]
                gi, e, G = groups[gidx]
                b = gidx % 2
                set_ = ui % 2
                ts = slice(tt * 512, (tt + 1) * 512)
                n = 8 * G * 128
                WG = WGb[b][:, 0:n].rearrange("p (k c) -> p k c", k=8)
                WU = WUb[b][:, 0:n].rearrange("p (k c) -> p k c", k=8)
                xk = [kxbf(c, tt) for c in range(8)]
                for g in range(G):
                    pg, kpg = psb("gu")
                    pu, kpu = psb("gu")
                    for kc in range(8):
                        mm(pg[:], WG[:, kc, g * 128:(g + 1) * 128], XBF[:, kc, ts], kc == 0, kc == 7, kWG[b] + xk, [kpg])
                    for kc in range(8):
                        mm(pu[:], WU[:, kc, g * 128:(g + 1) * 128], XBF[:, kc, ts], kc == 0, kc == 7, kWU[b] + xk, [kpu])
                    s32, ks32 = scb("fs", (0, 1, 2))
                    tk.op("act", lambda: A.activation(out=s32, in_=pg[:], func=AF.Silu), [kpg], [ks32])
                    if not moe:
                        tk.op("dve", lambda: V.tensor_tensor(out=HT(set_, g), in0=pu[:], in1=s32, op=ALU.mult), [kpu, ks32],
                              [KSL(21 + set_)])
                    else:
                        t32, kt32 = scb("ft", (3, 4, 5))
                        tk.op("dve", lambda: V.tensor_tensor(out=t32, in0=pu[:], in1=s32, op=ALU.mult), [kpu, ks32], [kt32])
                        tk.op("pool", lambda: G_.tensor_tensor(out=HT(set_, g), in0=t32, in1=GBC[:, ts], op=ALU.mult),
                              [kt32, "rope"], [KSL(21 + set_)])

            def DN(ui):
                gidx, tt = units[ui]
                gi, e, G = groups[gidx]
                b = gidx % 2
                set_ = ui % 2
                ts = slice(tt * 512, (tt + 1) * 512)
                WD = WDb[b][:, 0:G * 1024].rearrange("p (g c) -> p g c", g=G)
                for d in range(8):
                    py, kpy = psb("y")
                    for g in range(G):
                        mm(py[:], WD[:, g, d * 128:(d + 1) * 128], HT(set_, g), g == 0, g == G - 1, kWD[b] + [KSL(21 + set_)], [kpy])
                    tk.op("dve", lambda: V.tensor_tensor(out=X32[:, d, ts], in0=py[:], in1=X32[:, d, ts], op=ALU.add),
                          [kpy, kx32(d, tt)], [kx32(d, tt)])

            G_ = G
            load(0)
            cur_e = "none"
            for ui in range(len(units)):
                gidx, tt = units[ui]
                if ui == 0 and moe:
                    cur_e = groups[gidx][1]
                    build_gbc(cur_e)
                GU(ui)
                if moe and ui + 1 < len(units):
                    ne = groups[units[ui + 1][0]][1]
                    if ne != cur_e:
                        build_gbc(ne)
                        cur_e = ne
                if ui > 0:
                    DN(ui - 1)
                if tt == 0 and gidx + 1 < len(groups):
                    load(gidx + 1)
                if gidx == len(groups) - 1 and tt >= 2:
                    ln_tile(tt - 2, pb + 16, pb + 24, ln2_scale)
            DN(len(units) - 1)
            ln_tile(NT - 2, pb + 16, pb + 24, ln2_scale)
            ln_tile(NT - 1, pb + 16, pb + 24, ln2_scale)

        for s in range(nseq):
            for j in range(16):
                pair = nxt("xs", [0, 2])
                stg = SC[:, pair:pair + 2, :].rearrange("p a b -> p (a b)")
                kst = [("sc", pair), ("sc", pair + 1)]
                tk.dma("sp", stg, xin[s, j * 128:(j + 1) * 128, :], [], kst, ("xs", pair))
                for half in range(2):
                    pt_, kp = psb("pj")
                    for cc in range(4):
                        c = half * 4 + cc
                        tk.op("pe", lambda: T.transpose(pt_[:, cc * 128:(cc + 1) * 128], stg[:, c * 128:(c + 1) * 128], IDF[:]),
                              kst + ["idf"], [kp])
                    tt = j // 4
                    tk.op("act", lambda: A.activation(
                        out=X32[:, half * 4:half * 4 + 4, j * 128:(j + 1) * 128],
                        in_=pt_[:].rearrange("p (a b) -> p a b", a=4), func=AF.Copy),
                        [kp], [("x32j", half, j)] + [kx32(half * 4 + cc, tt) for cc in range(4)])
                if j % 4 == 3:
                    ln_tile(j // 4, 0, 8, ALPHA)
            for l in range(nlayers):
                if stop == "ln0":
                    break
                pb = 16 + l * NPL
                if mixer(s, l, pb):
                    break
                mark("ln1")
                if stop == "mix%d" % l:
                    break
                ffn(s, l, pb)
                mark("ln2")
                if stop == "ffn%d" % l:
                    break
            for j in range(16):
                tt = j // 4
                pair = nxt("yo", [0, 2])
                stg = SC[:, pair:pair + 2, :].rearrange("p a b -> p (a b)")
                kst = [("sc", pair), ("sc", pair + 1)]
                for half in range(2):
                    pt_, kp = psb("pj")
                    for cc in range(4):
                        c = half * 4 + cc
                        tk.op("pe", lambda: T.transpose(pt_[:, cc * 128:(cc + 1) * 128], X32[:, c, j * 128:(j + 1) * 128], IDF[:]),
                              [kx32(c, tt), ("x32j", half, j), "idf"], [kp])
                    tk.op("act", lambda: A.activation(out=stg[:, half * 512:(half + 1) * 512], in_=pt_[:], func=AF.Copy),
                          [kp], [kst[half]])
                tk.dma("sp", yout[s, j * 128:(j + 1) * 128, :], stg, kst, [("yout", s, j)], ("yo", pair))
        tk.finish()
    return nc


_PROG = {}


def kernel(**inputs):
    inp = {k: np.asarray(v) for k, v in inputs.items()}
    sh = prep_shared(inp)
    x = inp["x"]
    per = x.shape[0] // NCORES
    if per not in _PROG:
        _PROG[per] = build_program(nseq=per)
    nc = _PROG[per]
    in_maps = []
    for c in range(NCORES):
        m = dict(sh)
        m["xin"] = np.ascontiguousarray(x[per * c:per * (c + 1)], dtype=np.float32)
        in_maps.append(m)
    res = run_bass_kernel_spmd(nc, in_maps, core_ids=list(range(NCORES)))
    out = np.concatenate([np.asarray(r["yout"]) for r in res.results], axis=0)
    return out.astype(np.float32)
```

```python
import os
import numpy as np
from contextlib import ExitStack
import concourse.bass as bass
import concourse.mybir as mybir
from concourse.bass_utils import run_bass_kernel_spmd

F32 = mybir.dt.float32
BF16 = mybir.dt.bfloat16
AF = mybir.ActivationFunctionType
ALU = mybir.AluOpType
AX = mybir.AxisListType

D = 1024
S = 2048
DEPTH = 2
NCORES = 8
D_IN = 2656
D_FF = 2816
NEXP = 8
D_FFE = 3584
ALPHA = float((2 * DEPTH) ** 0.25)
LN_EPS = 1e-5
RMS_EPS = 1e-6
MASKVAL = -1.0e6
ATT_LAG = 3
NT = 4
NPL = 48
PA_COLS, PR_COLS, PC_COLS = 448, 384, 192
OFF_A = 0
OFF_R = 8 * PA_COLS
OFF_C = OFF_R + 5 * 8 * PR_COLS
WIN_TOT = OFF_C + 5 * 8 * PC_COLS
GAMMA = [1.0 - 2.0 ** (-5.0 - h) for h in range(5)]
SLOPES = [2.0 ** (-8.0 * (h + 1.0) / 5.0) for h in range(5)]


def _kcp(w):
    n = w.shape[1]
    return np.ascontiguousarray(w.reshape(8, 128, n).transpose(1, 0, 2))


def _swap(w):
    h = w.shape[1] // 2
    return np.concatenate([w[:, h:], w[:, :h]], axis=1)


def ffn_groups():
    gl = []
    f = 0
    while f < D_FF // 128:
        g = min(4, D_FF // 128 - f)
        gl.append((0, None, f, g))
        f += g
    for e in range(NEXP):
        for f in range(0, D_FFE // 128, 4):
            gl.append((1, e, f, 4))
    return gl


def prep_shared(inp):
    f32 = np.float32
    out = {}
    P = np.zeros((128, 16 + DEPTH * NPL), f32)

    def pc(v):
        return v.reshape(-1, 128).T

    P[:, 0:8] = pc(inp["ln_emb_g"])
    P[:, 8:16] = pc(inp["ln_emb_b"])
    for l in range(DEPTH):
        b = 16 + l * NPL
        P[:, b:b + 8] = pc(inp["ln1_g"][l])
        P[:, b + 8:b + 16] = pc(inp["ln1_b"][l])
        P[:, b + 16:b + 24] = pc(inp["ln2_g"][l])
        P[:, b + 24:b + 32] = pc(inp["ln2_b"][l])
        P[:, b + 32:b + 34] = pc(inp["q_norm_g"][l])
        P[:, b + 34:b + 35] = pc(inp["kv_norm_g"][l])
        beta = np.ones((1024,), f32)
        beta[0:384] = inp["beta_mla"][l]
        beta[704:1024] = inp["beta_moba"][l]
        P[:, b + 35:b + 43] = pc(beta)
    out["params"] = P
    winp = np.zeros((DEPTH, 128, WIN_TOT), f32)
    wuq = np.zeros((DEPTH, 128, 2 * 768), f32)
    wukv = np.zeros((DEPTH, 128, 1152), f32)
    wo = np.zeros((DEPTH, 128, 8 * 1024), f32)
    for l in range(DEPTH):
        w = inp["w_in"][l]
        pa = np.concatenate([w[:, 0:256], w[:, 256:384], w[:, 384:416], _swap(w[:, 384:416])], axis=1)
        winp[l, :, OFF_A:OFF_A + 8 * PA_COLS] = _kcp(pa).reshape(128, -1)
        for h in range(5):
            q = w[:, 416 + 64 * h:416 + 64 * h + 64]
            k = w[:, 736 + 64 * h:736 + 64 * h + 64]
            v = w[:, 1056 + 64 * h:1056 + 64 * h + 64]
            g = w[:, 1376 + 64 * h:1376 + 64 * h + 64]
            pr = np.concatenate([q, _swap(q), k, _swap(k), v, g], axis=1)
            o = OFF_R + h * 8 * PR_COLS
            winp[l, :, o:o + 8 * PR_COLS] = _kcp(pr).reshape(128, -1)
            mq = w[:, 1696 + 64 * h:1696 + 64 * h + 64]
            mk = w[:, 2016 + 64 * h:2016 + 64 * h + 64]
            mv = w[:, 2336 + 64 * h:2336 + 64 * h + 64]
            pcc = np.concatenate([mq, mk, mv], axis=1)
            o = OFF_C + h * 8 * PC_COLS
            winp[l, :, o:o + 8 * PC_COLS] = _kcp(pcc).reshape(128, -1)
        uq = inp["w_uq"][l]
        cols = []
        for h in range(6):
            nope = uq[:, h * 96:h * 96 + 64]
            rope = uq[:, h * 96 + 64:h * 96 + 96]
            cols += [rope, _swap(rope), nope]
        uqp = np.concatenate(cols, axis=1)
        wuq[l] = uqp.reshape(2, 128, 768).transpose(1, 0, 2).reshape(128, -1)
        ukv = inp["w_ukv"][l]
        kc = []
        vc = []
        for h in range(6):
            kc += [np.zeros((128, 64), f32), ukv[:, h * 128:h * 128 + 64]]
            vc += [ukv[:, h * 128 + 64:h * 128 + 128]]
        wukv[l] = np.concatenate(kc + vc, axis=1)
        wo[l] = _kcp(inp["w_o"][l]).reshape(128, -1)
    out["winp"] = winp
    out["wuq"] = wuq
    out["wukv"] = wukv
    out["wo"] = wo
    gl = ffn_groups()
    ffw = np.zeros((len(gl), 128, 12288), f32)
    for gi, (lay, e, f0, G) in enumerate(gl):
        if e is None:
            wg, wu, wd = inp["ffn_w_gate"][0], inp["ffn_w_up"][0], inp["ffn_w_down"][0]
        else:
            wg, wu, wd = inp["exp_w_gate"][0, e], inp["exp_w_up"][0, e], inp["exp_w_down"][0, e]
        c0, c1 = f0 * 128, (f0 + G) * 128
        ffw[gi, :, 0:8 * G * 128] = _kcp(wg[:, c0:c1]).reshape(128, -1)
        ffw[gi, :, 4096:4096 + 8 * G * 128] = _kcp(wu[:, c0:c1]).reshape(128, -1)
        ffw[gi, :, 8192:8192 + G * 1024] = wd[c0:c1, :].reshape(G, 128, 1024).transpose(1, 0, 2).reshape(128, -1)
    out["ffw"] = ffw
    out["router"] = _kcp(inp["router"][0]).reshape(128, 64)
    pos = np.arange(S, dtype=np.float64)
    inv_m = 10000.0 ** (-np.arange(0, 32, 2, dtype=np.float64) / 32)
    ang = inv_m[:, None] * pos[None, :]
    out["ropem"] = np.concatenate([np.cos(ang), np.cos(ang), -np.sin(ang), np.sin(ang)], axis=0).astype(f32)
    inv_r = 10000.0 ** (-np.arange(0, 64, 2, dtype=np.float64) / 64)
    ang = inv_r[:, None] * pos[None, :]
    out["roper"] = np.concatenate([np.cos(ang), np.cos(ang), -np.sin(ang), np.sin(ang)], axis=0).astype(f32)
    i = np.arange(128, dtype=np.float64)
    dec = np.zeros((5, 128, 512), f32)
    qdec = np.zeros((5, 64, 512), f32)
    kdec = np.zeros((128, 5), f32)
    for h in range(5):
        lg = np.log(GAMMA[h])
        dm = np.where(i[None, :] >= i[:, None], np.exp(-lg * (i[:, None] + 1.0)), 0.0) * 0.125
        dec[h] = np.tile(dm, (1, 4))
        qd = np.exp(lg * (i + 1.0))
        qdec[h] = np.tile(qd[None, :], (64, 4))
        kdec[:, h] = np.exp(lg * (127.0 - i)) * 0.125
    out["dec"] = dec
    out["qdec"] = qdec
    out["kdec"] = kdec
    al = np.zeros((128, 5 * 32), f32)
    for h in range(5):
        for c in range(32):
            al[:, h * 32 + c] = SLOPES[h] * (i + (c - 16) * 128.0)
    out["alibi"] = al
    b1 = np.zeros((8, S), f32)
    for n in range(8):
        b1[n, n * 256:(n + 1) * 256] = 1.0
    out["blk1h"] = b1
    cm = np.zeros((128, 256), f32)
    cm[:, 0:128] = np.eye(128)
    cm[:, 128:256] = (i[None, :] >= i[:, None]).astype(f32)
    out["cmisc"] = cm
    mc = np.zeros((128, 128), f32)
    for t in range(8):
        bq = (8 + t) // 2
        for n in range(8):
            mc[:, t * 8 + n] = 1.0 if n < bq else 0.0
            mc[:, 64 + t * 8 + n] = 0.0 if n < bq else -1.0e30
    out["mobc"] = mc
    return out


class TK:
    def __init__(self, nc, es):
        self.nc = nc
        self.es = es
        self.eng = {"pe": nc.tensor, "act": nc.scalar, "dve": nc.vector, "pool": nc.gpsimd, "sp": nc.sync}
        self.sem = {k: es.enter_context(nc.semaphore("s_" + k)) for k in self.eng}
        self.cnt = {k: 0 for k in self.eng}
        self.seen = {k: {} for k in self.eng}
        self.lastw = {}
        self.readers = {}
        self.dsem = {}

    def _wait(self, e, ev):
        sem, val, src = ev
        if src == "pe" and e == "pe":
            return
        if self.seen[e].get(src, 0) >= val:
            return
        self.eng[e].wait_ge(sem, val)
        self.seen[e][src] = val

    def _deps(self, e, reads, writes):
        for k in reads:
            ev = self.lastw.get(k)
            if ev:
                self._wait(e, ev)
            if isinstance(k, tuple) and k[0] == "ps":
                for ev in self.readers.get(k, {}).values():
                    if ev[2] != e:
                        self._wait(e, ev)
        for k in writes:
            ev = self.lastw.get(k)
            if ev:
                self._wait(e, ev)
            for ev in self.readers.get(k, {}).values():
                self._wait(e, ev)

    def _commit(self, ev, reads, writes):
        for k in reads:
            self.readers.setdefault(k, {})[ev[2]] = ev
        for k in writes:
            self.lastw[k] = ev
            self.readers[k] = {}

    def op(self, e, fn, reads=(), writes=()):
        self._deps(e, reads, writes)
        inst = fn()
        self.cnt[e] += 1
        inst.then_inc(self.sem[e], 1)
        self._commit((self.sem[e], self.cnt[e], e), reads, writes)

    def dma(self, q, out, in_, reads, writes, skey, **kw):
        self._deps(q, reads, writes)
        if skey not in self.dsem:
            self.dsem[skey] = [self.es.enter_context(self.nc.semaphore("d%d" % len(self.dsem))), 0]
        d = self.dsem[skey]
        src = "d:" + str(skey)
        if d[1] > 0:
            self._wait(q, (d[0], d[1], src))
        d[1] += 16
        self.eng[q].dma_start(out=out, in_=in_, **kw).then_inc(d[0], 16)
        self._commit((d[0], d[1], src), reads, writes)

    def finish(self):
        for skey, d in self.dsem.items():
            self._wait("sp", (d[0], d[1], "d:" + str(skey)))
        for e in ("pe", "act", "dve", "pool"):
            if self.cnt[e]:
                self._wait("sp", (self.sem[e], self.cnt[e], e))


MARKS = []


def build_program(nseq=2, nlayers=DEPTH, stop=None, taps=()):
    nc = bass.Bass("TRN2", target_bir_lowering=False)
    gl = ffn_groups()
    dr = {}

    def din(name, shape):
        dr[name] = nc.dram_tensor(name, list(shape), F32, kind="ExternalInput").ap()
        return dr[name]

    xin = din("xin", (nseq, S, D))
    params_d = din("params", (128, 16 + DEPTH * NPL))
    winp_d = din("winp", (DEPTH, 128, WIN_TOT))
    wuq_d = din("wuq", (DEPTH, 128, 1536))
    wukv_d = din("wukv", (DEPTH, 128, 1152))
    wo_d = din("wo", (DEPTH, 128, 8192))
    ffw_d = din("ffw", (len(gl), 128, 12288))
    router_d = din("router", (128, 64))
    ropem_d = din("ropem", (64, S))
    roper_d = din("roper", (128, S))
    dec_d = din("dec", (5, 128, 512))
    qdec_d = din("qdec", (5, 64, 512))
    kdec_d = din("kdec", (128, 5))
    alibi_d = din("alibi", (128, 160))
    blk1h_d = din("blk1h", (8, S))
    cmisc_d = din("cmisc", (128, 256))
    mobc_d = din("mobc", (128, 128))
    yout = nc.dram_tensor("yout", [nseq, S, D], F32, kind="ExternalOutput").ap()
    tap_d = {}
    for tname, tshape in taps:
        tap_d[tname] = nc.dram_tensor(tname, list(tshape), F32, kind="ExternalOutput").ap()

    es = ExitStack()
    with es:
        def sb(name, shape, dt):
            return es.enter_context(nc.sbuf_tensor(name, list(shape), dt))

        NSLAB = 27
        X32 = sb("X32", (128, 8, S), F32)
        SL = sb("SL", (128, NSLAB, S), BF16)
        ROPE = sb("ROPE", (128, S), F32)
        NSC = 7
        SC = sb("SC", (128, NSC, 512), F32)
        PAR = sb("PAR", (128, 16 + DEPTH * NPL), F32)
        PARA = sb("PARA", (128, 16 + DEPTH * NPL), F32)
        IDB = sb("IDB", (128, 128), BF16)
        TRI = sb("TRI", (128, 128), BF16)
        IDF = sb("IDF", (128, 128), F32)
        ONF = sb("ONF", (128, 128), F32)
        ONB = sb("ONB", (128, 128), BF16)
        ALI = sb("ALI", (128, 160), F32)
        KDEC = sb("KDEC", (128, 5), F32)
        DECB = sb("DECB", (128, 512), F32)
        QDECB = sb("QDECB", (64, 512), F32)
        ROUT = sb("ROUT", (128, 64), F32)
        GATE = sb("GATE", (128, 16, 8), F32)
        SM = sb("SM", (128, 128), F32)
        MOBC = sb("MOBC", (128, 128), F32)
        RALL = sb("RALL", (64, 17, 64), F32)
        KS = sb("KS", (64, 8), F32)
        PS = [es.enter_context(nc.psum_tensor("PS%d" % i, [128, 512], F32)) for i in range(8)]
        tk = TK(nc, es)
        V, A, G, T = nc.vector, nc.scalar, nc.gpsimd, nc.tensor

        rot = {}

        def nxt(name, lst):
            i = rot.get(name, 0)
            rot[name] = i + 1
            return lst[i % len(lst)]

        def psb(grp):
            banks = {"st": [0, 1, 2, 3], "ot": [4, 5], "pj": [6, 7], "aux": [7, 6], "gu": [0, 1, 2, 3], "y": [4, 5, 6]}[grp]
            b = nxt("ps_" + grp, banks)
            return PS[b], ("ps", b)

        def psf(b):
            return PS[b], ("ps", b)

        def scb(grp="r", lst=(4, 5, 6)):
            i = nxt("sc_" + grp, list(lst))
            return SC[:, i, :], ("sc", i)

        def scf(i):
            return SC[:, i, :], ("sc", i)

        def slab(i):
            return SL[:, i, :]

        def slabs(i, n):
            return SL[:, i:i + n, :].rearrange("p a b -> p (a b)")

        XBF0, MIX0 = 0, 8
        PT_SLAB = 16
        XBF = SL[:, 0:8, :]
        MIXT = SL[:, 8:16, :]

        def kx32(c, tt):
            return ("x32", c, tt)

        def kxbf(c, tt):
            return ("xbf", c, tt)

        tk.dma("sp", PAR[:], params_d[:, :], [], ["par"], "c")
        tk.dma("sp", IDF[:], cmisc_d[:, 0:128], [], ["idf"], "c")
        tk.dma("pool", IDB[:], cmisc_d[:, 0:128], [], ["idb"], "c")
        tk.dma("pool", TRI[:], cmisc_d[:, 128:256], [], ["tri"], "c")
        tk.dma("sp", ALI[:], alibi_d[:, :], [], ["ali"], "c")
        tk.dma("sp", KDEC[:], kdec_d[:, :], [], ["kdec"], "c")
        tk.dma("sp", ROUT[:], router_d[:, :], [], ["rout"], "c")
        tk.dma("sp", MOBC[:], mobc_d[:, :], [], ["mobc"], "c")
        tk.op("dve", lambda: V.memset(ONF[:], 1.0), [], ["onf"])
        tk.op("dve", lambda: V.tensor_scalar(out=PARA[:], in0=PAR[:], scalar1=ALPHA, scalar2=None, op0=ALU.mult), ["par"], ["par"])
        tk.op("dve", lambda: V.memset(ONB[:], 1.0), [], ["onb"])

        def tap(name, src_ap, keys, dst=None):
            if name in tap_d:
                tk.dma("sp", tap_d[name] if dst is None else dst, src_ap, keys, [("tap", name, str(dst is None))], ("tap", name))

        def layer_norm(gcol, bcol, out_scale):
            for tt in range(NT):
                ln_tile(tt, gcol, bcol, out_scale)

        def ln_tile(tt, gcol, bcol, out_scale):
            if True:
                ts = slice(tt * 512, (tt + 1) * 512)
                pm, kpm = psf(5)
                pq, kpq = psf(7)
                for c in range(8):
                    tk.op("pe", lambda c=c: T.matmul(pm[:], ONF[:], X32[:, c, ts], start=(c == 0), stop=(c == 7)),
                          [kx32(c, tt), "onf"], [kpm])
                for c in range(8):
                    pi = nxt("pt", [0, 1, 2, 3])
                    sq = SL[:, PT_SLAB, pi * 512:(pi + 1) * 512]
                    ksq = ("pt", pi)
                    tk.op("act", lambda c=c, sq=sq: A.activation(out=sq, in_=X32[:, c, ts], func=AF.Square),
                          [kx32(c, tt)], [ksq])
                    tk.op("pe", lambda c=c, sq=sq: T.matmul(pq[:], ONB[:], sq, start=(c == 0), stop=(c == 7)),
                          [ksq, "onb"], [kpq])
                mean, kmean = scf(0)
                var, kvar = scf(1)
                rs, krs = scf(2)
                nb, knb = scf(3)
                tk.op("act", lambda: A.activation(out=mean, in_=pm[:], func=AF.Copy, scale=1.0 / D), [kpm], [kmean])
                tk.op("dve", lambda: V.tensor_tensor(out=var, in0=mean, in1=mean, op=ALU.mult), [kmean], [kvar])
                tk.op("dve", lambda: V.scalar_tensor_tensor(out=var, in0=pq[:], scalar=1.0 / D, in1=var,
                                                            op0=ALU.mult, op1=ALU.subtract), [kpq, kvar], [kvar])
                tk.op("act", lambda: A.activation(out=var, in_=var, func=AF.Ln, bias=LN_EPS, scale=1.0), [kvar], [kvar])
                tk.op("act", lambda: A.activation(out=rs, in_=var, func=AF.Exp, scale=-0.5), [kvar], [krs])
                tk.op("dve", lambda: V.scalar_tensor_tensor(out=nb, in0=mean, scalar=-1.0, in1=rs,
                                                            op0=ALU.mult, op1=ALU.mult), [kmean, krs], [knb])
                PS_ = PAR if out_scale == 1.0 else PARA
                for c in range(8):
                    t1, kt1 = scb()
                    gc = PAR[:, gcol + c:gcol + c + 1]
                    bc = PAR[:, bcol + c:bcol + c + 1]
                    gs = PS_[:, gcol + c:gcol + c + 1]
                    bs_ = PS_[:, bcol + c:bcol + c + 1]
                    e1, e2 = ("dve", "dve") if c % 4 != 3 else ("pool", "pool")
                    E1 = V if e1 == "dve" else G
                    tk.op(e1, lambda: E1.tensor_tensor(out=t1, in0=X32[:, c, ts], in1=rs, op=ALU.mult),
                          [kx32(c, tt), krs], [kt1])
                    tk.op(e2, lambda: E1.tensor_tensor(out=t1, in0=t1, in1=nb, op=ALU.add), [knb, kt1], [kt1])
                    tk.op("act", lambda: A.activation(out=XBF[:, c, ts], in_=t1, func=AF.Identity, bias=bc, scale=gc),
                          [kt1, "par"], [kxbf(c, tt)])
                    tk.op("act", lambda: A.activation(out=X32[:, c, ts], in_=t1, func=AF.Identity, bias=bs_, scale=gs),
                          [kt1, "par"], [kx32(c, tt)])

        def attention(QT, kq, KT, kk, VA, kva, scale, bias_h, nsub, mix_chunk, mix_p0, bcol_unused=None):
            units = [(qt, j) for qt in range(NT) for j in range(4 * qt + 4)]
            state = {}

            def emit_st(qt, j):
                m = max(0, j - 4 * qt)
                c0 = m * 128
                st, kst = psb("st")
                tk.op("pe", lambda: T.matmul(st[:, c0:512], KT[:, j * 128:(j + 1) * 128],
                                             QT[:, qt * 512 + c0:(qt + 1) * 512], start=True, stop=True),
                      [kq, kk], [kst])
                pi = nxt("pt", [0, 1, 2, 3])
                pt = SL[:, PT_SLAB, pi * 512:(pi + 1) * 512]
                kpt = ("pt", pi)
                if bias_h is None:
                    tk.op("act", lambda: A.activation(out=pt[:, c0:512], in_=st[:, c0:512], func=AF.Exp, scale=scale),
                          [kst], [kpt])
                elif nsub == 1:
                    col = bias_h * 32 + 16 + (j - 4 * qt)
                    tk.op("act", lambda: A.activation(out=pt[:, c0:512], in_=st[:, c0:512], func=AF.Exp, scale=scale,
                                                      bias=ALI[:, col:col + 1]), [kst, "ali"], [kpt])
                else:
                    for mm in range(m, 4):
                        col = bias_h * 32 + 16 + (j - 4 * qt - mm)
                        tk.op("act", lambda mm=mm, col=col: A.activation(
                            out=pt[:, mm * 128:(mm + 1) * 128], in_=st[:, mm * 128:(mm + 1) * 128], func=AF.Exp,
                            scale=scale, bias=ALI[:, col:col + 1]), [kst, "ali"], [kpt])
                if j >= 4 * qt:
                    tk.op("pool", lambda: G.tensor_tensor(out=pt[:, c0:c0 + 128], in0=pt[:, c0:c0 + 128], in1=TRI[:],
                                                          op=ALU.mult), [kpt, "tri"], [kpt])
                return (qt, j, c0, pt, kpt)

            def emit_pv(u):
                qt, j, c0, pt, kpt = u
                if j == 0:
                    state["ot"] = psb("ot")
                ot, kot = state["ot"]
                nj = 4 * qt + 4
                tk.op("pe", lambda: T.matmul(ot[:, c0:512], VA[:, j, :], pt[:, c0:512], start=(j == 0), stop=(j == nj - 1),
                                             skip_group_check=True),
                      [kva, kpt], [kot])
                if j == nj - 1:
                    rc, krc = scb()
                    tk.op("dve", lambda: V.reciprocal(out=rc[0:64, :], in_=ot[64:128, :]), [kot], [krc])
                    ts = slice(qt * 512, (qt + 1) * 512)
                    tk.op("dve", lambda: V.tensor_tensor(out=MIXT[mix_p0:mix_p0 + 64, mix_chunk, ts], in0=ot[0:64, :],
                                                         in1=rc[0:64, :], op=ALU.mult),
                          [kot, krc], [("mix", mix_chunk, qt, mix_p0), ("sl", 8 + mix_chunk)])

            pend = []
            for (qt, j) in units:
                pend.append(emit_st(qt, j))
                if len(pend) > ATT_LAG:
                    emit_pv(pend.pop(0))
                yield
            while pend:
                emit_pv(pend.pop(0))

        def run_chain(items):
            p0, m0, h0 = items[0]
            for _ in p0(h0, h0 % 2):
                pass
            for i, (p, m, h) in enumerate(items):
                nx = None
                if i + 1 < len(items):
                    pn, mn, hn = items[i + 1]
                    nx = pn(hn, hn % 2)
                for _ in m(h, h % 2):
                    if nx is not None:
                        next(nx, None)
                if nx is not None:
                    for _ in nx:
                        pass

        def run_heads(nh, prep, attn):
            run_chain([(prep, attn, h) for h in range(nh)])

        PT_SLAB = 16

        def mix_loc(ch):
            return ch // 128, ch % 128

        def rms_bc(ps_list_keys, nparts, inv_n, out_rs, krs_out):
            pass

        def mm(out, lhsT, rhs, start, stop, reads, writes, **kw):
            tk.op("pe", lambda: T.matmul(out, lhsT, rhs, start=start, stop=stop, **kw), reads, writes)

        def mark(name):
            MARKS.append((name, tk.cnt["pe"]))

        def mixer(s, l, pb):
            fuse_ln = not (stop in ("mla1", "mla", "ret", "moba"))
            mark("mixer_start s%d l%d" % (s, l))
            KSL = lambda i: ("sl", i)
            XK = lambda tt: [kxbf(c, tt) for c in range(8)]
            CQN = SL[:, 17:19, :]
            CKVN = SL[:, 19, :]
            QT = SL[:, 20, :]
            KT = SL[:, 21, :]
            VA = SL[:, 22, :].rearrange("p (a b) -> p a b", a=16)
            PACKA = slabs(23, 2)[:, 0:3584].rearrange("p (k c) -> p k c", c=PA_COLS)
            WUQ = SL[:, 25, 0:1536].rearrange("p (k h c) -> p k h c", k=2, h=6)
            WUKV = SL[:, 26, 0:1152]
            tk.dma("pool", slabs(23, 2)[:, 0:3584], winp_d[l, :, OFF_A:OFF_A + 3584], [], [KSL(23), KSL(24)],
                   "wA", max_dma_last_dim=4096)
            stq = SC[:, 0:3, :].rearrange("p a b -> p (a b)")
            kstq = [("sc", 0), ("sc", 1), ("sc", 2)]
            tk.dma("sp", stq, wuq_d[l, :, :], [], kstq, "wB")
            for kc in range(2):
                tk.op("dve", lambda: V.tensor_scalar(out=SL[:, 25, kc * 768:(kc + 1) * 768], in0=stq[:, kc * 768:(kc + 1) * 768],
                                                     scalar1=PAR[:, pb + 32 + kc:pb + 33 + kc], scalar2=None, op0=ALU.mult),
                      kstq + ["par"], [KSL(25)])
            stk = SC[:, 3:6, :].rearrange("p a b -> p (a b)")
            kstk = [("sc", 3), ("sc", 4), ("sc", 5)]
            tk.dma("sp", stk[:, 0:1152], wukv_d[l, :, :], [], kstk, "wC")
            tk.op("dve", lambda: V.tensor_scalar(out=WUKV, in0=stk[:, 0:1152], scalar1=PAR[:, pb + 34:pb + 35], scalar2=None,
                                                 op0=ALU.mult), kstk + ["par"], [KSL(26)])
            tk.dma("sp", ROPE[0:64, :], ropem_d[:, :], [], ["rope"], "wD")
            tk.op("pool", lambda: G.memset(SL[32:64, 21, :], 0.0), [], [KSL(21)])
            tk.op("pool", lambda: G.memset(SL[32:64, 20, :], 0.0), [], [KSL(20)])
            tk.op("pool", lambda: G.memset(VA[:, :, 64:128], 1.0), [], [KSL(22)])

            def rope_to(ps_ap, kps, nrow, ts, out_ap, out_keys, also=None):
                a_, ka = scf(0)
                b_, kb = scf(1)
                tk.op("dve", lambda: V.tensor_tensor(out=a_[0:nrow, :], in0=ps_ap[0:nrow, :], in1=ROPE[0:nrow, ts], op=ALU.mult),
                      [kps, "rope"], [ka])
                tk.op("dve", lambda: V.tensor_tensor(out=b_[0:nrow, :], in0=ps_ap[nrow:2 * nrow, :], in1=ROPE[nrow:2 * nrow, ts],
                                                     op=ALU.mult), [kps, "rope"], [kb])
                if also is None:
                    tk.op("pool", lambda: G.tensor_tensor(out=out_ap, in0=a_[0:nrow, :], in1=b_[0:nrow, :], op=ALU.add),
                          [ka, kb], out_keys)
                else:
                    c_, kc_ = scf(2)
                    tk.op("pool", lambda: G.tensor_tensor(out=c_[0:nrow, :], in0=a_[0:nrow, :], in1=b_[0:nrow, :], op=ALU.add),
                          [ka, kb], [kc_])
                    tk.op("act", lambda: A.activation(out=out_ap, in_=c_[0:nrow, :], func=AF.Copy), [kc_], out_keys)
                    o2, k2, tab, ktab = also
                    tk.op("dve", lambda: V.tensor_tensor(out=o2, in0=c_[0:nrow, :], in1=tab, op=ALU.mult), [kc_, ktab], k2)

            for tt in range(NT):
                ts = slice(tt * 512, (tt + 1) * 512)
                pcs = []
                ssq, kssq = psf(6)
                for cc in range(2):
                    pc_, kpc = psf(4 + cc)
                    for kc in range(8):
                        mm(pc_[:], PACKA[:, kc, cc * 128:(cc + 1) * 128], XBF[:, kc, ts], kc == 0, kc == 7,
                           [KSL(23), KSL(24)] + XK(tt), [kpc])
                    sq, ksq = scb()
                    tk.op("act", lambda: A.activation(out=sq, in_=pc_[:], func=AF.Square), [kpc], [ksq])
                    mm(ssq[:], ONF[:], sq, cc == 0, cc == 1, [ksq, "onf"], [kssq])
                    pcs.append((pc_, kpc))
                sd, ksd = scf(0)
                tk.op("act", lambda: A.activation(out=sd, in_=ssq[:], func=AF.Ln, bias=RMS_EPS, scale=1.0 / 256), [kssq], [ksd])
                tk.op("act", lambda: A.activation(out=sd, in_=sd, func=AF.Exp, scale=-0.5), [ksd], [ksd])
                for cc in range(2):
                    pc_, kpc = pcs[cc]
                    tk.op("dve", lambda: V.tensor_tensor(out=CQN[:, cc, ts], in0=pc_[:], in1=sd, op=ALU.mult), [kpc, ksd],
                          [("cqn", cc, tt)])
                pk_, kpk = psf(0)
                for kc in range(8):
                    mm(pk_[:], PACKA[:, kc, 256:384], XBF[:, kc, ts], kc == 0, kc == 7, [KSL(23), KSL(24)] + XK(tt), [kpk])
                sq, ksq = scb()
                tk.op("act", lambda: A.activation(out=sq, in_=pk_[:], func=AF.Square), [kpk], [ksq])
                ss2, kss2 = psf(7)
                mm(ss2[:], ONF[:], sq, True, True, [ksq, "onf"], [kss2])
                sd2, ksd2 = scf(1)
                tk.op("act", lambda: A.activation(out=sd2, in_=ss2[:], func=AF.Ln, bias=RMS_EPS, scale=1.0 / 128), [kss2], [ksd2])
                tk.op("act", lambda: A.activation(out=sd2, in_=sd2, func=AF.Exp, scale=-0.5), [ksd2], [ksd2])
                tk.op("dve", lambda: V.tensor_tensor(out=CKVN[:, ts], in0=pk_[:], in1=sd2, op=ALU.mult), [kpk, ksd2],
                      [("ckvn", tt)])
                pr_, kpr = psf(1)
                for kc in range(8):
                    mm(pr_[0:64, :], PACKA[:, kc, 384:448], XBF[:, kc, ts], kc == 0, kc == 7, [KSL(23), KSL(24)] + XK(tt), [kpr])
                rope_to(pr_, kpr, 32, ts, KT[0:32, ts], [KSL(21)])

            def dump():
                for c in range(8):
                    for tt in range(NT):
                        ts = slice(tt * 512, (tt + 1) * 512)
                        tk.op("act", lambda: A.activation(out=X32[:, c, ts], in_=MIXT[:, c, ts], func=AF.Copy),
                              [("mix", c, tt, 0), ("mix", c, tt, 64)], [kx32(c, tt)])
                return True
            mark("mla_heads")
            if stop == "mla1":
                return dump()
            SETS = [(20, 21, 22), (13, 14, 15)]
            tk.op("act", lambda: A.activation(out=SL[0:32, 14, :], in_=SL[0:32, 21, :], func=AF.Copy), [KSL(21)], [KSL(14)])
            tk.op("pool", lambda: G.memset(SL[32:64, 14, :], 0.0), [], [KSL(14)])
            tk.op("pool", lambda: G.memset(SL[32:64, 13, :], 0.0), [], [KSL(13)])
            tk.op("pool", lambda: G.memset(SL[:, 15, :].rearrange("p (a b) -> p a b", a=16)[:, :, 64:128], 1.0), [], [KSL(15)])

            def mla_prep(h, bs):
                qs, ks_, vs = SETS[bs]
                QTb, KTb = SL[:, qs, :], SL[:, ks_, :]
                VAb = SL[:, vs, :].rearrange("p (a b) -> p a b", a=16)
                for tt in range(NT):
                    ts = slice(tt * 512, (tt + 1) * 512)
                    pk_, kpk = psb("pj")
                    mm(pk_[:], WUKV[:, h * 128:(h + 1) * 128], CKVN[:, ts], True, True, [KSL(26), ("ckvn", tt)], [kpk])
                    tk.op("act", lambda: A.activation(out=KTb[64:128, ts], in_=pk_[64:128, :], func=AF.Copy), [kpk], [KSL(ks_)])
                    pq_, kpq = psb("pj")
                    for kc in range(2):
                        mm(pq_[:], WUQ[:, kc, h, :], CQN[:, kc, ts], kc == 0, kc == 1, [KSL(25), ("cqn", kc, tt)], [kpq])
                    tk.op("act", lambda: A.activation(out=QTb[64:128, ts], in_=pq_[64:128, :], func=AF.Copy), [kpq], [KSL(qs)])
                    rope_to(pq_, kpq, 32, ts, QTb[0:32, ts], [KSL(qs)])
                    yield
                for half in range(2):
                    pv_, kpv = psb("aux")
                    for jj in range(8):
                        j = half * 8 + jj
                        mm(pv_[:, jj * 64:(jj + 1) * 64], CKVN[:, j * 128:(j + 1) * 128], WUKV[:, 768 + h * 64:768 + (h + 1) * 64],
                           True, True, [KSL(26), ("ckvn", j // 4)], [kpv], skip_group_check=True)
                    tk.op("dve", lambda: V.tensor_copy(out=VAb[:, half * 8:(half + 1) * 8, 0:64],
                                                       in_=pv_[:].rearrange("p (a b) -> p a b", a=8)), [kpv], [KSL(vs)])
                    yield

            def mla_attn(h, bs):
                qs, ks_, vs = SETS[bs]
                return attention(SL[:, qs, :], KSL(qs), SL[:, ks_, :], KSL(ks_),
                                 SL[:, vs, :].rearrange("p (a b) -> p a b", a=16), KSL(vs),
                                 float(96 ** -0.5), None, 1, h // 2, (h % 2) * 64)

            mark("ret")
            RSETS = [((17, 18), 19, 20, 22, 21), ((24, 25), 26, 14, 15, 23)]
            def ret_prep(h, bs):
                if h == 0:
                    tk.dma("sp", ROPE[:], roper_d[:, :], [], ["rope"], "wD")
                (p0_, p1_), qs, ks_, vs, rs_ = RSETS[bs]
                PACKR = slabs(p0_, 2)[:, 0:3072].rearrange("p (k c) -> p k c", c=PR_COLS)
                QTr, KTr = SL[:, qs, :], SL[:, ks_, :]
                VK = SL[:, vs, :].rearrange("p (a b) -> p a b", a=16)
                RB = SL[0:64, rs_, 0:1024].rearrange("p (a b) -> p a b", a=16)
                cd = float(GAMMA[h] ** 128)
                o = OFF_R + h * 3072
                kw_ = [KSL(p0_), KSL(p1_)]
                tk.dma("pool", slabs(p0_, 2)[:, 0:3072], winp_d[l, :, o:o + 3072], [], kw_, "wA", max_dma_last_dim=4096)
                for tt in range(NT):
                    ts = slice(tt * 512, (tt + 1) * 512)
                    pq_, kpq = psb("pj")
                    for kc in range(8):
                        mm(pq_[:], PACKR[:, kc, 0:128], XBF[:, kc, ts], kc == 0, kc == 7, kw_ + XK(tt), [kpq])
                    rope_to(pq_, kpq, 64, ts, QTr[0:64, ts], [KSL(qs)])
                    yield
                    pk_, kpk = psb("pj")
                    for kc in range(8):
                        mm(pk_[:], PACKR[:, kc, 128:256], XBF[:, kc, ts], kc == 0, kc == 7, kw_ + XK(tt), [kpk])
                    rope_to(pk_, kpk, 64, ts, KTr[0:64, ts], [KSL(ks_)])
                    yield
                for half in range(2):
                    pv_, kpv = psb("aux")
                    for jj in range(8):
                        j = half * 8 + jj
                        for kc in range(8):
                            mm(pv_[:, jj * 64:(jj + 1) * 64], XBF[:, kc, j * 128:(j + 1) * 128], PACKR[:, kc, 256:320],
                               kc == 0, kc == 7, kw_ + [kxbf(kc, j // 4)], [kpv], skip_group_check=True)
                    tk.op("act", lambda: A.activation(out=VK[:, half * 8:(half + 1) * 8, 0:64],
                                                      in_=pv_[:].rearrange("p (a b) -> p a b", a=8), func=AF.Copy),
                          [kpv], [KSL(vs)])
                    yield
                    pd_, kpd = psb("aux")
                    for jj in range(8):
                        j = half * 8 + jj
                        mm(pd_[:, jj * 64:(jj + 1) * 64], KTr[0:64, j * 128:(j + 1) * 128], IDB[0:64, 0:64], True, True,
                           [KSL(ks_), "idb"], [kpd], skip_group_check=True)
                    tk.op("dve", lambda: V.tensor_scalar(out=VK[:, half * 8:(half + 1) * 8, 64:128],
                                                         in0=pd_[:].rearrange("p (a b) -> p a b", a=8),
                                                         scalar1=KDEC[:, h:h + 1], scalar2=None, op0=ALU.mult),
                          [kpd, "kdec"], [KSL(vs)])
                    yield
                pkv = [psb("aux"), psb("aux")]
                for n in range(16):
                    pb_, kpb = pkv[n // 8]
                    mm(pb_[0:64, (n % 8) * 64:(n % 8 + 1) * 64], VK[:, n, 64:128], VK[:, n, 0:64], True, True, [KSL(vs)], [kpb],
                       skip_group_check=True)
                tk.op("dve", lambda: V.memset(RALL[:, 0, :], 0.0), [], ["rall"])
                for n in range(15):
                    pb_, kpb = pkv[n // 8]
                    tk.op("dve", lambda: V.scalar_tensor_tensor(out=RALL[:, n + 1, :], in0=RALL[:, n, :], scalar=cd,
                                                                in1=pb_[0:64, (n % 8) * 64:(n % 8 + 1) * 64],
                                                                op0=ALU.mult, op1=ALU.add), [kpb, "rall"], ["rall"])
                tk.op("act", lambda: A.activation(out=RB, in_=RALL[:, 0:16, :], func=AF.Copy), ["rall"], [KSL(rs_)])
                yield

            def ret_main(h, bs):
                (p0_, p1_), qs, ks_, vs, rs_ = RSETS[bs]
                PACKR = slabs(p0_, 2)[:, 0:3072].rearrange("p (k c) -> p k c", c=PR_COLS)
                QTr, KTr = SL[:, qs, :], SL[:, ks_, :]
                VK = SL[:, vs, :].rearrange("p (a b) -> p a b", a=16)
                RB = SL[0:64, rs_, 0:1024].rearrange("p (a b) -> p a b", a=16)
                kw_ = [KSL(p0_), KSL(p1_)]
                ch = 384 + 64 * h
                mchunk, mp0 = ch // 128, ch % 128
                tk.dma("sp", DECB[:], dec_d[h, :, :], [], ["decb"], "wD")
                tk.dma("sp", QDECB[:], qdec_d[h, :, :], [], ["qdecb"], "wD")
                for tt in range(NT):
                    ts = slice(tt * 512, (tt + 1) * 512)
                    pa_, kpa = psb("st")
                    for nn in range(4):
                        n = tt * 4 + nn
                        mm(pa_[:, nn * 128:(nn + 1) * 128], KTr[0:64, n * 128:(n + 1) * 128], QTr[0:64, n * 128:(n + 1) * 128],
                           True, True, [KSL(qs), KSL(ks_)], [kpa], skip_group_check=True)
                    pi = nxt("pt", [0, 1, 2, 3])
                    at = SL[:, PT_SLAB, pi * 512:(pi + 1) * 512]
                    kat = ("pt", pi)
                    tk.op("dve", lambda: V.tensor_tensor(out=at, in0=pa_[:], in1=DECB[:], op=ALU.mult), [kpa, "decb"], [kat])
                    pg_, kpg = psb("pj")
                    for kc in range(8):
                        mm(pg_[0:64, :], PACKR[:, kc, 320:384], XBF[:, kc, ts], kc == 0, kc == 7, kw_ + XK(tt), [kpg])
                    sg, ksg = scf(6)
                    tk.op("act", lambda: A.activation(out=sg[0:64, :], in_=pg_[0:64, :], func=AF.Silu), [kpg], [ksg])
                    yield
                    po_, kpo = psb("ot")
                    for nn in range(4):
                        n = tt * 4 + nn
                        cs = slice(nn * 128, (nn + 1) * 128)
                        mm(po_[0:64, cs], VK[:, n, 0:64], at[:, cs], True, False, [KSL(vs), kat], [kpo], skip_group_check=True)
                        mm(po_[0:64, cs], RB[:, n, :], QTr[0:64, n * 128:(n + 1) * 128], False, True, [KSL(rs_), KSL(qs)], [kpo],
                           skip_group_check=True)
                    o32, ko32 = scf(3)
                    cen, kcen = scf(4)
                    zz, kzz = scf(5)
                    tk.op("dve", lambda: V.tensor_tensor(out=o32[0:64, :], in0=po_[0:64, :], in1=QDECB[:], op=ALU.mult),
                          [kpo, "qdecb"], [ko32])
                    yield
                    pm_, kpm = psb("aux")
                    mm(pm_[0:64, :], ONF[0:64, 0:64], o32[0:64, :], True, True, [ko32, "onf"], [kpm])
                    tk.op("dve", lambda: V.scalar_tensor_tensor(out=cen[0:64, :], in0=pm_[0:64, :], scalar=-1.0 / 64,
                                                                in1=o32[0:64, :], op0=ALU.mult, op1=ALU.add),
                          [kpm, ko32], [kcen])
                    pi2 = nxt("pt", [0, 1, 2, 3])
                    zb = SL[:, PT_SLAB, pi2 * 512:(pi2 + 1) * 512]
                    kzb = ("pt", pi2)
                    tk.op("act", lambda: A.activation(out=zb[0:64, :], in_=cen[0:64, :], func=AF.Square), [kcen], [kzb])
                    yield
                    pv2, kpv2 = psb("aux")
                    mm(pv2[0:64, :], ONB[0:64, 0:64], zb[0:64, :], True, True, [kzb, "onb"], [kpv2])
                    tk.op("act", lambda: A.activation(out=zz[0:64, :], in_=pv2[0:64, :], func=AF.Ln, bias=RMS_EPS,
                                                      scale=1.0 / 64), [kpv2], [kzz])
                    tk.op("act", lambda: A.activation(out=zz[0:64, :], in_=zz[0:64, :], func=AF.Exp, scale=-0.5), [kzz], [kzz])
                    tk.op("dve", lambda: V.tensor_tensor(out=cen[0:64, :], in0=cen[0:64, :], in1=zz[0:64, :], op=ALU.mult),
                          [kcen, kzz], [kcen])
                    tk.op("pool", lambda: G.tensor_tensor(out=MIXT[mp0:mp0 + 64, mchunk, ts], in0=cen[0:64, :], in1=sg[0:64, :],
                                                          op=ALU.mult), [kcen, ksg], [("mix", mchunk, tt, mp0), ("sl", 8 + mchunk)])
                    yield

            run_chain([(mla_prep, mla_attn, h) for h in range(6)] + [(ret_prep, ret_main, h) for h in range(5)])

            mark("moba")
            if stop == "ret":
                return dump()
            MSETS = [(20, 21, 22, 17, 0), (23, 24, 25, 18, 2)]
            for (qs, ks_, vs, ps_, sc0) in MSETS:
                tk.op("pool", lambda: G.memset(SL[64:96, ks_, :], 0.0), [], [KSL(ks_)])
                tk.dma("pool", SL[64:72, ks_, :], blk1h_d[:, :], [], [KSL(ks_)], "wA", max_dma_last_dim=4096)
                tk.op("pool", lambda: G.memset(SL[64:96, qs, :], 0.0), [], [KSL(qs)])
                tk.op("pool", lambda: G.memset(SL[:, vs, :].rearrange("p (a b) -> p a b", a=16)[:, :, 64:128], 1.0), [], [KSL(vs)])
            MB3 = SL[:, 26, 0:1024].rearrange("p (t c) -> p t c", t=8)
            tk.op("pool", lambda: G.memset(MB3, 0.0), [], ["mb", KSL(26)])

            def moba_prep(h, bs):
                qs, ks_, vs, ps_, sc0 = MSETS[bs]
                QTb, KTb = SL[:, qs, :], SL[:, ks_, :]
                VAb = SL[:, vs, :].rearrange("p (a b) -> p a b", a=16)
                PACKC = SL[:, ps_, 0:1536].rearrange("p (k c) -> p k c", c=PC_COLS)
                o = OFF_C + h * 1536
                tk.dma("pool", SL[:, ps_, 0:1536], winp_d[l, :, o:o + 1536], [], [KSL(ps_)], "wA")
                kw_ = [KSL(ps_)]
                q32 = {}
                for tt in range(NT):
                    ts = slice(tt * 512, (tt + 1) * 512)
                    pq_, kpq = psb("pj")
                    for kc in range(8):
                        mm(pq_[:, :], PACKC[:, kc, 0:128], XBF[:, kc, ts], kc == 0, kc == 7, kw_ + XK(tt), [kpq])
                    tk.op("dve", lambda: V.tensor_copy(out=QTb[0:64, ts], in_=pq_[0:64, :]), [kpq], [KSL(qs)])
                    tk.op("dve", lambda: V.tensor_copy(out=KTb[0:64, ts], in_=pq_[64:128, :]), [kpq], [KSL(ks_)])
                    if tt >= 2:
                        q32[tt] = scf(sc0 + tt - 2)
                        tk.op("dve", lambda: V.tensor_copy(out=q32[tt][0][0:64, :], in_=pq_[0:64, :]), [kpq], [q32[tt][1]])
                    tk.op("dve", lambda: V.tensor_reduce(out=KS[:, 2 * tt:2 * tt + 2],
                                                         in_=pq_[64:128, :].rearrange("p (a b) -> p a b", a=2),
                                                         axis=AX.X, op=ALU.add), [kpq], ["ks"])
                    yield
                for half in range(2):
                    pv_, kpv = psb("aux")
                    for jj in range(8):
                        j = half * 8 + jj
                        for kc in range(8):
                            mm(pv_[:, jj * 64:(jj + 1) * 64], XBF[:, kc, j * 128:(j + 1) * 128], PACKC[:, kc, 128:192],
                               kc == 0, kc == 7, kw_ + [kxbf(kc, j // 4)], [kpv], skip_group_check=True)
                    tk.op("dve", lambda: V.tensor_copy(out=VAb[:, half * 8:(half + 1) * 8, 0:64],
                                                       in_=pv_[:].rearrange("p (a b) -> p a b", a=8)), [kpv], [KSL(vs)])
                    yield
                pg_, kpg = psb("aux")
                for t in range(8):
                    i = 8 + t
                    tt = i // 4
                    mm(pg_[:, t * 8:(t + 1) * 8], q32[tt][0][0:64, (i % 4) * 128:(i % 4 + 1) * 128], KS[:, 0:8], True, True,
                       [q32[tt][1], "ks"], [kpg], skip_group_check=True)
                gp = SM[:, 0:64]
                gp3 = gp.rearrange("p (t n) -> p t n", t=8)
                cnt3 = SM[:, 64:128].rearrange("p (t n) -> p t n", t=8)
                tk.op("dve", lambda: V.tensor_tensor(out=gp, in0=pg_[:, 0:64], in1=MOBC[:, 0:64], op=ALU.mult), [kpg, "mobc"], ["sm0"])
                tk.op("dve", lambda: V.tensor_tensor(out=gp, in0=gp, in1=MOBC[:, 64:128], op=ALU.add), ["sm0", "mobc"], ["sm0"])
                cmp_, kcmp = scf(6)
                cmp4 = cmp_.rearrange("p (t n m) -> p t n m", t=8, n=8)
                tk.op("dve", lambda: V.tensor_tensor(out=cmp4, in0=gp3.unsqueeze(2).broadcast_to([128, 8, 8, 8]),
                                                     in1=gp3.unsqueeze(3).broadcast_to([128, 8, 8, 8]), op=ALU.is_gt),
                      ["sm0"], [kcmp])
                tk.op("dve", lambda: V.tensor_reduce(out=cnt3, in_=cmp4, axis=AX.X, op=ALU.add), [kcmp], ["sm1"])
                tk.op("dve", lambda: V.tensor_scalar(out=SM[:, 64:128], in0=SM[:, 64:128], scalar1=3.0, scalar2=MASKVAL,
                                                     op0=ALU.is_ge, op1=ALU.mult), ["sm1"], ["sm1"])
                tk.op("dve", lambda: V.tensor_tensor(out=MB3[:, :, 64:72], in0=cnt3,
                                                     in1=MOBC[:, 0:64].rearrange("p (t n) -> p t n", t=8), op=ALU.mult),
                      ["sm1", "mobc"], ["mb"])
                for k2 in range(2):
                    pm_, kpm = psb("aux")
                    for t4 in range(4):
                        t = k2 * 4 + t4
                        mm(pm_[:, t4 * 128:(t4 + 1) * 128], MB3[:, t, :], IDB[:], True, True, ["mb", "idb"], [kpm],
                           skip_group_check=True)
                    tk.op("dve", lambda: V.tensor_copy(out=QTb[64:72, (8 + 4 * k2) * 128:(12 + 4 * k2) * 128], in_=pm_[64:72, :]),
                          [kpm], [KSL(qs)])
                yield

            def moba_attn(h, bs):
                qs, ks_, vs, ps_, sc0 = MSETS[bs]
                ch = 704 + 64 * h
                return attention(SL[0:96, qs, :], KSL(qs), SL[0:96, ks_, :], KSL(ks_),
                                 SL[:, vs, :].rearrange("p (a b) -> p a b", a=16), KSL(vs),
                                 0.125, h, 4 if h == 0 else 1, ch // 128, ch % 128)

            run_heads(5, moba_prep, moba_attn)

            mark("wo")
            if stop == "moba":
                return dump()
            WO = SL[:, 17:21, :].rearrange("p a b -> p (a b)").rearrange("p (k c) -> p k c", k=8)
            for kc in range(8):
                pair = nxt("wostg", [0, 2])
                stg = SC[:, pair:pair + 2, :].rearrange("p a b -> p (a b)")
                kst = [("sc", pair), ("sc", pair + 1)]
                tk.dma("sp", stg, wo_d[l, :, kc * 1024:(kc + 1) * 1024], [], kst, ("wB" if pair == 0 else "wC"))
                tk.op("dve", lambda: V.tensor_scalar(out=WO[:, kc, :], in0=stg, scalar1=PAR[:, pb + 35 + kc:pb + 36 + kc],
                                                     scalar2=None, op0=ALU.mult), kst + ["par"], [KSL(17 + kc // 2)])
            kwo = [KSL(17), KSL(18), KSL(19), KSL(20)]
            for tt in range(NT + 1):
                if tt >= 1 and fuse_ln:
                    ln_tile(tt - 1, pb + 0, pb + 8, ALPHA)
                    if l % 2 == 1:
                        router_tile(tt - 1)
                if tt == NT:
                    break
                ts = slice(tt * 512, (tt + 1) * 512)
                pa_, kpa = psf(6)
                pc_, kpc = psf(7)

                def sqmm(ps_, kps, c, p0, p1, first, last):
                    pi = nxt("sq21", [0, 1, 2, 3])
                    sq = SL[:, 21, pi * 512:(pi + 1) * 512]
                    ksq = ("sq21", pi)
                    tk.op("act", lambda: A.activation(out=sq[p0:p1, :], in_=MIXT[p0:p1, c, ts], func=AF.Square),
                          [("mix", c, tt, 0), ("mix", c, tt, 64), ("sl", 8 + c)], [ksq, ("sl", 21)])
                    mm(ps_[:], ONB[p0:p1, :], sq[p0:p1, :], first, last, [ksq, "onb"], [kps])
                for c in range(3):
                    sqmm(pa_, kpa, c, 0, 128, c == 0, c == 2)
                sqmm(pc_, kpc, 5, 64, 128, True, False)
                sqmm(pc_, kpc, 6, 0, 128, False, False)
                sqmm(pc_, kpc, 7, 0, 128, False, True)
                ra, kra = scf(4)
                rc, krc = scf(5)
                tk.op("act", lambda: A.activation(out=ra, in_=pa_[:], func=AF.Ln, bias=RMS_EPS, scale=1.0 / 384), [kpa], [kra])
                tk.op("act", lambda: A.activation(out=rc, in_=pc_[:], func=AF.Ln, bias=RMS_EPS, scale=1.0 / 320), [kpc], [krc])
                tk.op("act", lambda: A.activation(out=ra, in_=ra, func=AF.Exp, scale=-0.5), [kra], [kra])
                tk.op("act", lambda: A.activation(out=rc, in_=rc, func=AF.Exp, scale=-0.5), [krc], [krc])
                for d in range(8):
                    ds_ = slice(d * 128, (d + 1) * 128)
                    b3 = [nxt("w3", [0, 1, 2, 3, 4, 5]) for _ in range(3)]
                    (pA, kA), (pB, kB), (pC, kC) = [psf(b) for b in b3]
                    mk = lambda c: [("mix", c, tt, 0), ("mix", c, tt, 64), ("sl", 8 + c)]
                    for c in range(3):
                        mm(pA[:], WO[:, c, ds_], MIXT[:, c, ts], c == 0, c == 2, kwo + mk(c), [kA])
                    mm(pB[:], WO[:, 3, ds_], MIXT[:, 3, ts], True, False, kwo + mk(3), [kB])
                    mm(pB[:], WO[:, 4, ds_], MIXT[:, 4, ts], False, False, kwo + mk(4), [kB])
                    mm(pB[:], WO[0:64, 5, ds_], MIXT[0:64, 5, ts], False, True, kwo + mk(5), [kB])
                    mm(pC[:], WO[64:128, 5, ds_], MIXT[64:128, 5, ts], True, False, kwo + mk(5), [kC])
                    mm(pC[:], WO[:, 6, ds_], MIXT[:, 6, ts], False, False, kwo + mk(6), [kC])
                    mm(pC[:], WO[:, 7, ds_], MIXT[:, 7, ts], False, True, kwo + mk(7), [kC])
                    tk.op("dve", lambda: V.tensor_tensor(out=X32[:, d, ts], in0=pB[:], in1=X32[:, d, ts], op=ALU.add),
                          [kB, kx32(d, tt)], [kx32(d, tt)])
                    t1, kt1 = scb("wo", (0, 1, 2, 3))
                    tk.op("dve", lambda: V.tensor_tensor(out=t1, in0=pA[:], in1=ra, op=ALU.mult), [kA, kra], [kt1])
                    tk.op("pool", lambda: G.tensor_tensor(out=X32[:, d, ts], in0=X32[:, d, ts], in1=t1, op=ALU.add),
                          [kt1, kx32(d, tt)], [kx32(d, tt)])
                    t2, kt2 = scb("wo", (0, 1, 2, 3))
                    tk.op("dve", lambda: V.tensor_tensor(out=t2, in0=pC[:], in1=rc, op=ALU.mult), [kC, krc], [kt2])
                    tk.op("pool", lambda: G.tensor_tensor(out=X32[:, d, ts], in0=X32[:, d, ts], in1=t2, op=ALU.add),
                          [kt2, kx32(d, tt)], [kx32(d, tt)])

        def router_tile(tt):
            for j in range(4 * tt, 4 * tt + 4):
                pl, kpl = psb("aux")
                for kc in range(8):
                    mm(pl[:, 0:8], X32[:, kc, j * 128:(j + 1) * 128], ROUT[:, kc * 8:(kc + 1) * 8], kc == 0, kc == 7,
                       [kx32(kc, j // 4), "rout"], [kpl])
                lg = SM[:, 16:24]
                mx = SM[:, 24:32]
                tk.op("dve", lambda: V.tensor_scalar(out=lg, in0=pl[:, 0:8], scalar1=1.0 / ALPHA, scalar2=None, op0=ALU.mult),
                      [kpl], ["smr"])
                tk.op("dve", lambda: V.max(out=mx, in_=lg), ["smr"], ["smr"])
                tk.op("dve", lambda: V.tensor_tensor(out=SM[:, 32:33], in0=mx[:, 1:2], in1=mx[:, 0:1], op=ALU.subtract),
                      ["smr"], ["smr"])
                tk.op("act", lambda: A.activation(out=SM[:, 33:34], in_=SM[:, 32:33], func=AF.Exp), ["smr"], ["smr"])
                tk.op("dve", lambda: V.tensor_scalar(out=SM[:, 34:35], in0=SM[:, 33:34], scalar1=1.0, scalar2=None, op0=ALU.add),
                      ["smr"], ["smr"])
                tk.op("dve", lambda: V.reciprocal(out=SM[:, 35:36], in_=SM[:, 34:35]), ["smr"], ["smr"])
                tk.op("dve", lambda: V.tensor_tensor(out=SM[:, 36:37], in0=SM[:, 33:34], in1=SM[:, 35:36], op=ALU.mult),
                      ["smr"], ["smr"])
                tk.op("dve", lambda: V.tensor_scalar(out=SM[:, 40:48], in0=lg, scalar1=mx[:, 0:1], scalar2=SM[:, 35:36],
                                                     op0=ALU.is_equal, op1=ALU.mult), ["smr"], ["smr"])
                tk.op("dve", lambda: V.tensor_scalar(out=SM[:, 48:56], in0=lg, scalar1=mx[:, 1:2], scalar2=SM[:, 36:37],
                                                     op0=ALU.is_equal, op1=ALU.mult), ["smr"], ["smr"])
                tk.op("dve", lambda: V.tensor_tensor(out=GATE[:, j, :], in0=SM[:, 40:48], in1=SM[:, 48:56], op=ALU.add),
                      ["smr"], ["gate"])


        def ffn(s, l, pb):
            ln2_scale = 1.0 if l == DEPTH - 1 else ALPHA
            mark("ffn s%d l%d" % (s, l))
            KSL = lambda i: ("sl", i)
            groups = [(gi, e, G) for gi, (lay, e, f0, G) in enumerate(gl) if lay == l]
            moe = (l == 1)
            WGb = [slabs(8, 2), slabs(10, 2)]
            WUb = [slabs(12, 2), slabs(14, 2)]
            WDb = [slabs(17, 2), slabs(19, 2)]
            kWG = [[KSL(8), KSL(9)], [KSL(10), KSL(11)]]
            kWU = [[KSL(12), KSL(13)], [KSL(14), KSL(15)]]
            kWD = [[KSL(17), KSL(18)], [KSL(19), KSL(20)]]
            GBC = ROPE

            def HT(set_, g):
                return SL[:, 21 + set_, g * 512:(g + 1) * 512]

            def build_gbc(e):
                for q4 in range(4):
                    pgb, kpgb = psb("aux")
                    dg, kdg = scf(6)
                    for jj in range(4):
                        j = q4 * 4 + jj
                        cs = slice(jj * 128, (jj + 1) * 128)
                        tk.op("dve", lambda: V.tensor_scalar(out=dg[:, cs], in0=IDF[:], scalar1=GATE[:, j, e:e + 1], scalar2=None,
                                                             op0=ALU.mult), ["gate", "idf"], [kdg])
                        mm(pgb[:, cs], ONF[:], dg[:, cs], True, True, [kdg, "onf"], [kpgb], skip_group_check=True)
                    tk.op("act", lambda: A.activation(out=GBC[:, q4 * 512:(q4 + 1) * 512], in_=pgb[:], func=AF.Copy),
                          [kpgb], ["rope"])

            def load(gidx):
                gi, e, G = groups[gidx]
                b = gidx % 2
                n = 8 * G * 128
                tk.dma("pool", WGb[b][:, 0:n], ffw_d[gi, :, 0:n], [], kWG[b], ("fg", b), max_dma_last_dim=4096)
                tk.dma("pool", WUb[b][:, 0:n], ffw_d[gi, :, 4096:4096 + n], [], kWU[b], ("fu", b), max_dma_last_dim=4096)
                tk.dma("pool", WDb[b][:, 0:G * 1024], ffw_d[gi, :, 8192:8192 + G * 1024], [], kWD[b], ("fd", b),
                       max_dma_last_dim=4096)

            units = [(gidx, tt) for gidx in range(len(groups)) for tt in range(NT)]

            def GU(ui):
                gidx, tt = units[ui]
                gi, e, G = groups[gidx]
                b = gidx % 2
                set_ = ui % 3
                ts = slice(tt * 512, (tt + 1) * 512)
                n = 8 * G * 128
                WG = WGb[b][:, 0:n].rearrange("p (k c) -> p k c", k=8)
                WU = WUb[b][:, 0:n].rearrange("p (k c) -> p k c", k=8)
                xk = [kxbf(c, tt) for c in range(8)]
                for g in range(G):
                    pg, kpg = psb("gu")
                    pu, kpu = psb("gu")
                    for kc in range(8):
                        mm(pg[:], WG[:, kc, g * 128:(g + 1) * 128], XBF[:, kc, ts], kc == 0, kc == 7, kWG[b] + xk, [kpg])
                    for kc in range(8):
                        mm(pu[:], WU[:, kc, g * 128:(g + 1) * 128], XBF[:, kc, ts], kc == 0, kc == 7, kWU[b] + xk, [kpu])
                    s32, ks32 = scb("fs", (0, 1, 2))
                    tk.op("act", lambda: A.activation(out=s32, in_=pg[:], func=AF.Silu), [kpg], [ks32])
                    if not moe:
                        tk.op("dve", lambda: V.tensor_tensor(out=HT(set_, g), in0=pu[:], in1=s32, op=ALU.mult), [kpu, ks32],
                              [KSL(21 + set_)])
                    else:
                        t32, kt32 = scb("ft", (3, 4, 5))
                        tk.op("dve", lambda: V.tensor_tensor(out=t32, in0=pu[:], in1=s32, op=ALU.mult), [kpu, ks32], [kt32])
                        tk.op("pool", lambda: G_.tensor_tensor(out=HT(set_, g), in0=t32, in1=GBC[:, ts], op=ALU.mult),
                              [kt32, "rope"], [KSL(21 + set_)])

            def DN(ui):
                gidx, tt = units[ui]
                gi, e, G = groups[gidx]
                b = gidx % 2
                set_ = ui % 3
                ts = slice(tt * 512, (tt + 1) * 512)
                WD = WDb[b][:, 0:G * 1024].rearrange("p (g c) -> p g c", g=G)
                for d in range(8):
                    py, kpy = psb("y")
                    for g in range(G):
                        mm(py[:], WD[:, g, d * 128:(d + 1) * 128], HT(set_, g), g == 0, g == G - 1, kWD[b] + [KSL(21 + set_)], [kpy])
                    tk.op("dve", lambda: V.tensor_tensor(out=X32[:, d, ts], in0=py[:], in1=X32[:, d, ts], op=ALU.add),
                          [kpy, kx32(d, tt)], [kx32(d, tt)])

            G_ = G
            load(0)
            cur_e = "none"
            for ui in range(len(units)):
                gidx, tt = units[ui]
                if tt == 0:
                    e = groups[gidx][1]
                    if moe and e != cur_e:
                        build_gbc(e)
                        cur_e = e
                GU(ui)
                if ui > 1:
                    DN(ui - 2)
                if tt == (1 if gidx > 0 else 0) and gidx + 1 < len(groups):
                    load(gidx + 1)
                if gidx == len(groups) - 1 and tt >= 2:
                    ln_tile(tt - 2, pb + 16, pb + 24, ln2_scale)
            DN(len(units) - 2)
            DN(len(units) - 1)
            ln_tile(NT - 2, pb + 16, pb + 24, ln2_scale)
            ln_tile(NT - 1, pb + 16, pb + 24, ln2_scale)

        for s in range(nseq):
            for j in range(16):
                pair = nxt("xs", [0, 2])
                stg = SC[:, pair:pair + 2, :].rearrange("p a b -> p (a b)")
                kst = [("sc", pair), ("sc", pair + 1)]
                tk.dma("sp", stg, xin[s, j * 128:(j + 1) * 128, :], [], kst, ("xs", pair))
                for half in range(2):
                    pt_, kp = psb("pj")
                    for cc in range(4):
                        c = half * 4 + cc
                        tk.op("pe", lambda: T.transpose(pt_[:, cc * 128:(cc + 1) * 128], stg[:, c * 128:(c + 1) * 128], IDF[:]),
                              kst + ["idf"], [kp])
                    tt = j // 4
                    tk.op("act", lambda: A.activation(
                        out=X32[:, half * 4:half * 4 + 4, j * 128:(j + 1) * 128],
                        in_=pt_[:].rearrange("p (a b) -> p a b", a=4), func=AF.Copy),
                        [kp], [("x32j", half, j)] + [kx32(half * 4 + cc, tt) for cc in range(4)])
                if j % 4 == 3:
                    ln_tile(j // 4, 0, 8, ALPHA)
            for l in range(nlayers):
                if stop == "ln0":
                    break
                pb = 16 + l * NPL
                if mixer(s, l, pb):
                    break
                mark("ln1")
                if stop == "mix%d" % l:
                    break
                ffn(s, l, pb)
                mark("ln2")
                if stop == "ffn%d" % l:
                    break
            for j in range(16):
                tt = j // 4
                pair = nxt("yo", [0, 2])
                stg = SC[:, pair:pair + 2, :].rearrange("p a b -> p (a b)")
                kst = [("sc", pair), ("sc", pair + 1)]
                for half in range(2):
                    pt_, kp = psb("pj")
                    for cc in range(4):
                        c = half * 4 + cc
                        tk.op("pe", lambda: T.transpose(pt_[:, cc * 128:(cc + 1) * 128], X32[:, c, j * 128:(j + 1) * 128], IDF[:]),
                              [kx32(c, tt), ("x32j", half, j), "idf"], [kp])
                    tk.op("act", lambda: A.activation(out=stg[:, half * 512:(half + 1) * 512], in_=pt_[:], func=AF.Copy),
                          [kp], [kst[half]])
                tk.dma("sp", yout[s, j * 128:(j + 1) * 128, :], stg, kst, [("yout", s, j)], ("yo", pair))
        tk.finish()
    return nc


_PROG = {}


def kernel(**inputs):
    inp = {k: np.asarray(v) for k, v in inputs.items()}
    sh = prep_shared(inp)
    x = inp["x"]
    per = x.shape[0] // NCORES
    if per not in _PROG:
        _PROG[per] = build_program(nseq=per)
    nc = _PROG[per]
    in_maps = []
    for c in range(NCORES):
        m = dict(sh)
        m["xin"] = np.ascontiguousarray(x[per * c:per * (c + 1)], dtype=np.float32)
        in_maps.append(m)
    res = run_bass_kernel_spmd(nc, in_maps, core_ids=list(range(NCORES)))
    out = np.concatenate([np.asarray(r["yout"]) for r in res.results], axis=0)
    return out.astype(np.float32)
```
